# Optimizing a Trainium2 kernel written in Bass

```python
import jax, jax.numpy as jnp
from jax import lax
import numpy as np

D_MODEL = 1024
BATCH = 8
SEQ = 8192
DEPTH = 2

N_MIXERS = 2
EPS = 1e-6
GLA_HEADS = 4
GLA_DK = D_MODEL // 2 // GLA_HEADS
GLA_DV = D_MODEL // GLA_HEADS
GLA_GATE_RANK = 16
GLA_GATE_TAU = 16.0
GLA_CHUNK = 64
GLA_IN = 2 * GLA_HEADS * GLA_DK + GLA_HEADS * GLA_DV + GLA_GATE_RANK + GLA_HEADS * GLA_DV
MLA_HEADS = 8
MLA_NOPE = 128
MLA_ROPE = 64
MLA_V = D_MODEL // MLA_HEADS
MLA_Q_LORA = D_MODEL // 4
MLA_KV_LORA = D_MODEL // 8
MLA_IN = MLA_Q_LORA + MLA_KV_LORA + MLA_ROPE
MLA_SCALE = (MLA_NOPE + MLA_ROPE) ** -0.5
ROPE_THETA = 10000.0
Q_BLOCK = 128
MOE_GROUPS = 8
MOE_PER_GROUP = 8
MOE_EXPERTS = MOE_GROUPS * MOE_PER_GROUP
MOE_TOPK = 2
MOE_FF = D_MODEL // 4
MOE_BLOCK = 128

kernel_name = 'hybrid_gla_mla_hier_moe'


def rmsnorm(x, g):
    x32 = x.astype(jnp.float32)
    y = x32 * lax.rsqrt(jnp.mean(x32 * x32, axis=-1, keepdims=True) + EPS)
    return (y * g.astype(jnp.float32)).astype(x.dtype)


def rope_tables(positions):
    inv_freq = 1.0 / (ROPE_THETA ** (jnp.arange(0, MLA_ROPE, 2, dtype=jnp.float32) / MLA_ROPE))
    ang = positions.astype(jnp.float32)[..., None] * inv_freq
    return jnp.cos(ang), jnp.sin(ang)


def apply_rope(t, cos, sin):
    t32 = t.astype(jnp.float32)
    t1, t2 = jnp.split(t32, 2, axis=-1)
    return jnp.concatenate([t1 * cos - t2 * sin, t2 * cos + t1 * sin], axis=-1).astype(t.dtype)


def gla_mixer(h, w_in, w_gate, b_gate, out_norm, w_o):
    bsz, seq, _ = h.shape
    nc = seq // GLA_CHUNK
    qk = GLA_HEADS * GLA_DK
    vd = GLA_HEADS * GLA_DV
    proj = h @ w_in
    q, k, v, a_lr, r = jnp.split(proj, [qk, 2 * qk, 2 * qk + vd, 2 * qk + vd + GLA_GATE_RANK], axis=-1)
    log_a = jax.nn.log_sigmoid((a_lr @ w_gate + b_gate).astype(jnp.float32)) / GLA_GATE_TAU

    def chunks(t, d):
        return t.reshape(bsz, nc, GLA_CHUNK, GLA_HEADS, d).transpose(0, 3, 1, 2, 4).astype(jnp.float32)

    qc = chunks(q, GLA_DK) * (GLA_DK ** -0.5)
    kc = chunks(k, GLA_DK)
    vc = chunks(v, GLA_DV)
    g = jnp.cumsum(chunks(log_a, GLA_DK), axis=3)
    g_mid = g[:, :, :, GLA_CHUNK // 2:GLA_CHUNK // 2 + 1]
    g_last = g[:, :, :, -1:]
    causal = jnp.tril(jnp.ones((GLA_CHUNK, GLA_CHUNK), dtype=bool))
    a_intra = jnp.einsum('bhnid,bhnjd->bhnij', qc * jnp.exp(g - g_mid), kc * jnp.exp(g_mid - g))
    o_intra = jnp.einsum('bhnij,bhnjv->bhniv', jnp.where(causal, a_intra, 0.0), vc)
    q_inter = jnp.moveaxis(qc * jnp.exp(g), 2, 0)
    k_state = jnp.moveaxis(kc * jnp.exp(g_last - g), 2, 0)
    decay = jnp.moveaxis(jnp.exp(g_last[:, :, :, 0]), 2, 0)
    v_s = jnp.moveaxis(vc, 2, 0)

    def step(state, inp):
        q_i, k_i, v_i, d_i = inp
        o_i = jnp.einsum('bhid,bhdv->bhiv', q_i, state)
        state = state * d_i[..., None] + jnp.einsum('bhjd,bhjv->bhdv', k_i, v_i)
        return state, o_i

    s0 = jnp.zeros((bsz, GLA_HEADS, GLA_DK, GLA_DV), jnp.float32)
    _, o_inter = lax.scan(step, s0, (q_inter, k_state, v_s, decay))
    o = o_intra + jnp.moveaxis(o_inter, 0, 2)
    o = o.transpose(0, 2, 3, 1, 4).reshape(bsz, seq, GLA_HEADS, GLA_DV)
    o = o * lax.rsqrt(jnp.mean(o * o, axis=-1, keepdims=True) + EPS) * out_norm.astype(jnp.float32)
    o = o.reshape(bsz, seq, vd) * jax.nn.silu(r.astype(jnp.float32))
    return o.astype(h.dtype) @ w_o


def mla_mixer(h, cos, sin, w_in, q_norm, w_uq, kv_norm, w_uk, w_uv, w_o):
    bsz, seq, _ = h.shape
    nqb = seq // Q_BLOCK
    proj = h @ w_in
    c_q, c_kv, k_rope = jnp.split(proj, [MLA_Q_LORA, MLA_Q_LORA + MLA_KV_LORA], axis=-1)
    c_q = rmsnorm(c_q, q_norm)
    c_kv = rmsnorm(c_kv, kv_norm)
    q = jnp.einsum('bsc,chd->bshd', c_q, w_uq)
    q_nope, q_rope = q[..., :MLA_NOPE], q[..., MLA_NOPE:]
    q_rope = apply_rope(q_rope, cos[:, :, None, :], sin[:, :, None, :])
    k_rope = apply_rope(k_rope, cos, sin)
    q_lat = jnp.einsum('bshn,chn->bshc', q_nope, w_uk)
    q_cat = jnp.concatenate([q_lat, q_rope], axis=-1) * MLA_SCALE
    k_cat = jnp.concatenate([c_kv, k_rope], axis=-1)
    q_blocks = q_cat.reshape(bsz, nqb, Q_BLOCK, MLA_HEADS, MLA_KV_LORA + MLA_ROPE).transpose(1, 0, 2, 3, 4)
    k_pos = jnp.arange(seq)

    def attend(args):
        q_b, b_idx = args
        s = jnp.einsum('bqhc,bkc->bhqk', q_b, k_cat).astype(jnp.float32)
        q_pos = b_idx * Q_BLOCK + jnp.arange(Q_BLOCK)
        s = jnp.where(k_pos[None, :] <= q_pos[:, None], s, -jnp.inf)
        p = jax.nn.softmax(s, axis=-1).astype(c_kv.dtype)
        return jnp.einsum('bhqk,bkc->bqhc', p, c_kv)

    o_lat = lax.map(attend, (q_blocks, jnp.arange(nqb)))
    o_lat = o_lat.transpose(1, 0, 2, 3, 4).reshape(bsz, seq, MLA_HEADS, MLA_KV_LORA)
    o = jnp.einsum('bshc,chv->bshv', o_lat, w_uv).reshape(bsz, seq, MLA_HEADS * MLA_V)
    return o @ w_o


def hier_moe(h, w_group, w_expert, w1, w3, w2):
    bsz, seq, dm = h.shape
    n_tok = bsz * seq
    xf = h.reshape(n_tok, dm)
    tok = jnp.arange(n_tok)
    g_logits = (xf @ w_group).astype(jnp.float32)
    g_prob = jax.nn.softmax(g_logits, axis=-1)
    g_sel = jnp.argmax(g_logits, axis=-1)
    g_w = g_prob[tok, g_sel]
    e_logits = (xf @ w_expert).astype(jnp.float32).reshape(n_tok, MOE_GROUPS, MOE_PER_GROUP)
    e_prob = jax.nn.softmax(e_logits[tok, g_sel], axis=-1)
    top_p, top_i = lax.top_k(e_prob, MOE_TOPK)
    gates = g_w[:, None] * top_p / jnp.sum(top_p, axis=-1, keepdims=True)
    expert_id = g_sel[:, None] * MOE_PER_GROUP + top_i
    n_asg = n_tok * MOE_TOPK
    flat_e = expert_id.reshape(n_asg)
    flat_tok = jnp.arange(n_asg) // MOE_TOPK
    flat_gate = gates.reshape(n_asg)
    order = jnp.argsort(flat_e)
    se, stok, sgate = flat_e[order], flat_tok[order], flat_gate[order]
    counts = jax.ops.segment_sum(jnp.ones_like(flat_e), flat_e, num_segments=MOE_EXPERTS)
    starts = jnp.cumsum(counts) - counts
    padded = (counts + MOE_BLOCK - 1) // MOE_BLOCK * MOE_BLOCK
    pend = jnp.cumsum(padded)
    pstarts = pend - padded
    dest = pstarts[se] + (jnp.arange(n_asg) - starts[se])
    n_slots = n_asg + MOE_EXPERTS * MOE_BLOCK
    n_blocks = n_slots // MOE_BLOCK
    slot_tok = jnp.full((n_slots,), n_tok, dtype=jnp.int32).at[dest].set(stok.astype(jnp.int32))
    slot_gate = jnp.zeros((n_slots,), jnp.float32).at[dest].set(sgate)
    block_e = jnp.minimum(jnp.searchsorted(pend, jnp.arange(n_blocks) * MOE_BLOCK, side='right'), MOE_EXPERTS - 1)
    x_pad = jnp.concatenate([xf, jnp.zeros((1, dm), xf.dtype)], axis=0)
    xs = x_pad[slot_tok].reshape(n_blocks, MOE_BLOCK, dm)

    def expert_block(args):
        xb, e = args
        hid = jax.nn.silu(xb @ w1[e]) * (xb @ w3[e])
        return hid @ w2[e]

    ys = lax.map(expert_block, (xs, block_e)).reshape(n_slots, dm)
    ys = ys * slot_gate[:, None].astype(ys.dtype)
    out = jnp.zeros((n_tok + 1, dm), ys.dtype).at[slot_tok].add(ys)[:n_tok]
    return out.reshape(bsz, seq, dm)


def setup_inputs(seed: int = 0) -> dict:
    key = jax.random.key(seed)
    ks = jax.random.split(key, 24)
    f32 = jnp.float32
    n_gla = (DEPTH + N_MIXERS - 1) // N_MIXERS
    n_mla = DEPTH // N_MIXERS

    def nrm(k, shape, fan_in):
        return jax.random.normal(k, shape, f32) * (fan_in ** -0.5)

    def gain(k, shape):
        return 1.0 + 0.1 * jax.random.normal(k, shape, f32)

    x = jax.random.normal(ks[0], (BATCH, SEQ, D_MODEL), f32)
    offsets = jax.random.randint(ks[1], (BATCH, 1), 0, 4096, dtype=jnp.int32)
    positions = offsets + jnp.arange(SEQ, dtype=jnp.int32)[None, :]
    return {
        'x': x,
        'positions': positions,
        'attn_norm': gain(ks[2], (DEPTH, D_MODEL)),
        'ffn_norm': gain(ks[3], (DEPTH, D_MODEL)),
        'final_norm': gain(ks[4], (D_MODEL,)),
        'gla_w_in': nrm(ks[5], (n_gla, D_MODEL, GLA_IN), D_MODEL),
        'gla_w_gate': nrm(ks[6], (n_gla, GLA_GATE_RANK, GLA_HEADS * GLA_DK), GLA_GATE_RANK),
        'gla_b_gate': 0.1 * jax.random.normal(ks[7], (n_gla, GLA_HEADS * GLA_DK), f32),
        'gla_out_norm': gain(ks[8], (n_gla, GLA_DV)),
        'gla_w_o': nrm(ks[9], (n_gla, GLA_HEADS * GLA_DV, D_MODEL), GLA_HEADS * GLA_DV),
        'mla_w_in': nrm(ks[10], (n_mla, D_MODEL, MLA_IN), D_MODEL),
        'mla_q_norm': gain(ks[11], (n_mla, MLA_Q_LORA)),
        'mla_w_uq': nrm(ks[12], (n_mla, MLA_Q_LORA, MLA_HEADS, MLA_NOPE + MLA_ROPE), MLA_Q_LORA),
        'mla_kv_norm': gain(ks[13], (n_mla, MLA_KV_LORA)),
        'mla_w_uk': nrm(ks[14], (n_mla, MLA_KV_LORA, MLA_HEADS, MLA_NOPE), MLA_KV_LORA),
        'mla_w_uv': nrm(ks[15], (n_mla, MLA_KV_LORA, MLA_HEADS, MLA_V), MLA_KV_LORA),
        'mla_w_o': nrm(ks[16], (n_mla, MLA_HEADS * MLA_V, D_MODEL), MLA_HEADS * MLA_V),
        'moe_w_group': nrm(ks[17], (DEPTH, D_MODEL, MOE_GROUPS), D_MODEL),
        'moe_w_expert': nrm(ks[18], (DEPTH, D_MODEL, MOE_EXPERTS), D_MODEL),
        'moe_w1': nrm(ks[19], (DEPTH, MOE_EXPERTS, D_MODEL, MOE_FF), D_MODEL),
        'moe_w3': nrm(ks[20], (DEPTH, MOE_EXPERTS, D_MODEL, MOE_FF), D_MODEL),
        'moe_w2': nrm(ks[21], (DEPTH, MOE_EXPERTS, MOE_FF, D_MODEL), MOE_FF),
    }


def reference(x, positions, attn_norm, ffn_norm, final_norm,
              gla_w_in, gla_w_gate, gla_b_gate, gla_out_norm, gla_w_o,
              mla_w_in, mla_q_norm, mla_w_uq, mla_kv_norm, mla_w_uk, mla_w_uv, mla_w_o,
              moe_w_group, moe_w_expert, moe_w1, moe_w3, moe_w2):
    cos, sin = rope_tables(positions)
    for i in range(DEPTH):
        j = i // N_MIXERS
        h = rmsnorm(x, attn_norm[i])
        if i % N_MIXERS == 0:
            m = gla_mixer(h, gla_w_in[j], gla_w_gate[j], gla_b_gate[j], gla_out_norm[j], gla_w_o[j])
        else:
            m = mla_mixer(h, cos, sin, mla_w_in[j], mla_q_norm[j], mla_w_uq[j], mla_kv_norm[j],
                          mla_w_uk[j], mla_w_uv[j], mla_w_o[j])
        x = x + m.astype(x.dtype)
        h = rmsnorm(x, ffn_norm[i])
        x = x + hier_moe(h, moe_w_group[i], moe_w_expert[i], moe_w1[i], moe_w3[i], moe_w2[i]).astype(x.dtype)
    return rmsnorm(x, final_norm)
```

```python
import contextlib
import math
import numpy as np
import concourse.bass as bass
import concourse.mybir as mybir
from concourse.bass_utils import run_bass_kernel_spmd

F32 = mybir.dt.float32
BF16 = mybir.dt.bfloat16
I32 = mybir.dt.int32
ALU = mybir.AluOpType
AF = mybir.ActivationFunctionType
AX = mybir.AxisListType

SAME_ENGINE_SYNC = True
D = 1024
EPS = 1e-6
NE = 64


class Buf:
    __slots__ = ("name", "w", "r")

    def __init__(self, name=""):
        self.name = name
        self.w = None
        self.r = {}


class Sched:
    ENG_NAMES = ("pe", "act", "dve", "pool", "sp")

    def __init__(self, nc, stack, n_dma=(16, 12, 6)):
        self.nc = nc
        self.engs = {"pe": nc.tensor, "act": nc.scalar, "dve": nc.vector,
                     "pool": nc.gpsimd, "sp": nc.sync}
        self.sems = {}
        self.cnt = {}
        for e in self.ENG_NAMES:
            self.sems[e] = stack.enter_context(nc.semaphore("s_" + e))
            self.cnt[e] = 0
        self.dma_pool = {}
        self.dma_rr = {}
        for q, n in zip(("sp", "pool", "act"), n_dma):
            keys = []
            for i in range(n):
                k = "d_%s_%d" % (q, i)
                self.sems[k] = stack.enter_context(nc.semaphore(k))
                self.cnt[k] = 0
                keys.append(k)
            self.dma_pool[q] = keys
            self.dma_rr[q] = 0
        self.seen = {e: {} for e in self.ENG_NAMES}
        self.n_wait = 0
        self.n_ins = 0

    def _wait(self, eng, tickets):
        need = {}
        seen = self.seen[eng]
        for t in tickets:
            if t is None:
                continue
            s, v = t[0], t[1]
            if s == eng and (eng == "pe" or not SAME_ENGINE_SYNC or (SAME_ENGINE_SYNC == "raw" and len(t) > 2)):
                continue
            if seen.get(s, 0) >= v:
                continue
            if need.get(s, 0) < v:
                need[s] = v
        for s, v in need.items():
            self.engs[eng].wait_ge(self.sems[s], v)
            seen[s] = v
            self.n_wait += 1

    def _deps(self, reads, writes):
        tk = []
        for b in reads:
            tk.append(b.w)
        for b in writes:
            if b.w is not None:
                tk.append((b.w[0], b.w[1], "waw"))
            for s, v in b.r.items():
                tk.append((s, v, "war"))
        return tk

    def _commit(self, tk, reads, writes):
        s, v = tk
        for b in reads:
            if b.r.get(s, 0) < v:
                b.r[s] = v
        for b in writes:
            b.w = tk
            b.r = {}

    def op(self, eng, fn, reads=(), writes=()):
        self._wait(eng, self._deps(reads, writes))
        ins = fn(self.engs[eng])
        self.cnt[eng] += 1
        ins.then_inc(self.sems[eng], 1)
        tk = (eng, self.cnt[eng])
        self._commit(tk, reads, writes)
        self.n_ins += 1
        return tk

    def dma(self, q, out, in_, reads=(), writes=(), indirect=None, **kw):
        keys = self.dma_pool[q]
        k = keys[self.dma_rr[q] % len(keys)]
        self.dma_rr[q] += 1
        deps = self._deps(reads, writes)
        if self.cnt[k]:
            deps.append((k, self.cnt[k]))
        self._wait(q, deps)
        if indirect is None:
            ins = self.engs[q].dma_start(out=out, in_=in_, **kw)
        else:
            ins = self.engs[q].indirect_dma_start(out=out, in_=in_, **indirect)
        self.cnt[k] += 16
        ins.then_inc(self.sems[k], 16)
        tk = (k, self.cnt[k])
        self._commit(tk, reads, writes)
        self.n_ins += 1
        return tk

    def all_tickets(self):
        return [(k, v) for k, v in self.cnt.items() if v]

    def barrier(self):
        tk = self.all_tickets()
        for e in self.ENG_NAMES:
            self._wait(e, tk)

    def fence(self, eng, bufs_in, buf_out):
        tk = []
        for b in bufs_in:
            tk.append(b.w)
            for s, v in b.r.items():
                tk.append((s, v))
        self._wait(eng, tk)
        return self.op(eng, lambda e: e.memset(self.fence_t[:], 0.0), writes=[buf_out, self.fence_b])

    def finish(self):
        self._wait("sp", self.all_tickets())


def make_consts(S, BLK):
    NT = S // 128
    NSLOT = 2 * S + NE * BLK
    NB = NSLOT // BLK
    j = np.arange(128)[:, None]
    i = np.arange(128)[None, :]
    same = (j // 64) == (i // 64)
    Tinc = ((j <= i) & same).astype(np.float32)
    mid = (i // 64) * 64 + 32
    Tmid = ((j <= mid) & same).astype(np.float32)
    mats = {
        "ident": np.eye(128, dtype=np.float32),
        "TG": -Tinc / 16.0,
        "TQ": -(Tinc - Tmid) / 16.0,
        "TAFT": -((j > i) & same).astype(np.float32) / 16.0,
        "MASK": Tinc,
        "TRI": (j <= i).astype(np.float32),
        "SLT": (j < i).astype(np.float32),
        "ONES": np.ones((128, 128), np.float32),
    }
    cs = np.zeros((128, 128), np.float32)
    cs[:64, 0] = -1.0 / 16.0
    cs[64:, 1] = -1.0 / 16.0
    mats["CSG"] = cs
    misc = np.zeros((128, 128), np.float32)
    p = np.arange(128)
    inv_freq = 1.0 / (10000.0 ** (np.arange(0, 64, 2, dtype=np.float32) / 64.0))
    misc[:, 0] = inv_freq.astype(np.float32)[p % 32]
    misc[:, 1] = np.where((p % 64) < 32, -1.0, 1.0)
    misc[:, 2] = p
    misc[:, 3] = p + 128
    mats["misc"] = misc
    names = list(mats.keys())
    cmat = np.concatenate([mats[n] for n in names], axis=1).astype(np.float32)
    off = {n: k * 128 for k, n in enumerate(names)}
    thr = np.broadcast_to((np.arange(16, dtype=np.float32) * BLK)[None, :], (128, 16))
    tblk = np.broadcast_to((np.arange(NB, dtype=np.float32) * BLK)[None, :], (128, NB))
    iota = np.broadcast_to(np.arange(64, dtype=np.float32)[None, :], (128, 64))
    SPB = BLK // 128
    yrow = (np.arange(NB * SPB, dtype=np.float32)[None, :] * 128 + np.arange(128, dtype=np.float32)[:, None])
    cvec = np.concatenate([thr, tblk, iota, yrow], axis=1).astype(np.float32).copy()
    tokidx = (np.arange(NT, dtype=np.int32)[None, :] * 128 + np.arange(128, dtype=np.int32)[:, None]).astype(np.int32)
    return cmat, off, cvec, tokidx


def build_nc(S, BLK, dbg=None):
    NT = S // 128
    NG = S // 512
    NSLOT = 2 * S + NE * BLK
    NB = NSLOT // BLK
    SPB = BLK // 128
    cmat_np, coff, cvec_np, tokidx_np = make_consts(S, BLK)
    NCM = cmat_np.shape[1]
    NCV = cvec_np.shape[1]

    nc = bass.Bass("TRN2", target_bir_lowering=False)

    def din(name, shape, dt=F32):
        return nc.dram_tensor(name, list(shape), dt, kind="ExternalInput").ap()

    x_d = din("x", [S, D])
    pos_d = din("positions", [S], I32)
    attn_norm_d = din("attn_norm", [2, D])
    ffn_norm_d = din("ffn_norm", [2, D])
    final_norm_d = din("final_norm", [D])
    gla_w_in_d = din("gla_w_in", [1, D, 3088])
    gla_w_gate_d = din("gla_w_gate", [1, 16, 512])
    gla_b_gate_d = din("gla_b_gate", [1, 512])
    gla_out_norm_d = din("gla_out_norm", [1, 256])
    gla_w_o_d = din("gla_w_o", [1, D, D])
    mla_w_in_d = din("mla_w_in", [1, D, 448])
    mla_q_norm_d = din("mla_q_norm", [1, 256])
    mla_w_uq_d = din("mla_w_uq", [1, 256, 8, 192])
    mla_kv_norm_d = din("mla_kv_norm", [1, 128])
    mla_w_uk_d = din("mla_w_uk", [1, 128, 8, 128])
    mla_w_uv_d = din("mla_w_uv", [1, 128, 8, 128])
    mla_w_o_d = din("mla_w_o", [1, D, D])
    moe_w_group_d = din("moe_w_group", [2, D, 8])
    moe_w_expert_d = din("moe_w_expert", [2, D, 64])
    moe_w1_d = din("moe_w1", [2, 64, D, 256])
    moe_w3_d = din("moe_w3", [2, 64, D, 256])
    moe_w2_d = din("moe_w2", [2, 64, 256, D])
    cmat_d = din("cmat", [128, NCM])
    cvec_d = din("cvec", [128, NCV])
    tokidx_d = din("tokidx", [128, NT], I32)
    out_d = nc.dram_tensor("out", [S, D], F32, kind="ExternalOutput").ap()

    def dscr(name, shape, dt=F32):
        return nc.dram_tensor(name, list(shape), dt, kind="Internal").ap()

    X1_d = dscr("X1s", [S, D])
    X3_d = dscr("X3s", [S, D])
    H2_d = [dscr("H2s%d" % l, [S + 128, D], BF16) for l in range(2)]
    Y_d = [dscr("Ys%d" % l, [NSLOT, D], BF16) for l in range(2)]
    REC_d = [dscr("RECs%d" % l, [NSLOT, 2], I32) for l in range(2)]

    top = contextlib.ExitStack()
    with top:
        S_ = Sched(nc, top)

        uniq = [0]

        def sbt(stack, name, shape, dt):
            uniq[0] += 1
            return stack.enter_context(nc.sbuf_tensor("%s_u%d" % (name, uniq[0]), list(shape), dt))

        S_.fence_t = sbt(top, "fence_t", [128, 1], F32)
        S_.fence_b = Buf("fence")

        def V(fn, r=(), w=()):
            return S_.op("dve", fn, r, w)

        def A(fn, r=(), w=()):
            return S_.op("act", fn, r, w)

        def P(fn, r=(), w=()):
            return S_.op("pe", fn, r, w)

        def G(fn, r=(), w=()):
            return S_.op("pool", fn, r, w)

        ps = [top.enter_context(nc.psum_tensor("ps%d" % i, [128, 512], F32)) for i in range(8)]
        psB = [Buf("ps%d" % i) for i in range(8)]
        ps_rr = [0]

        def bank(avoid=()):
            while True:
                i = ps_rr[0] % 8
                ps_rr[0] += 1
                if i not in avoid:
                    return ps[i], psB[i]

        cm = sbt(top, "cm", [128, NCM], F32)
        cmB = Buf("cm")
        S_.dma("sp", cm[:], cmat_d, writes=[cmB])
        cmb = sbt(top, "cmb", [128, NCM], BF16)
        cmbB = Buf("cmb")
        V(lambda e: e.tensor_copy(cmb[:], cm[:]), [cmB], [cmbB])
        cv = sbt(top, "cv", [128, NCV], F32)
        cvB = Buf("cv")
        S_.dma("sp", cv[:], cvec_d, writes=[cvB])
        tokidx = sbt(top, "tokidx_sb", [128, NT], I32)
        tokidxB = Buf("tokidx")
        S_.dma("sp", tokidx[:], tokidx_d, writes=[tokidxB])

        def CM(name, n=128):
            return cm[:, coff[name]:coff[name] + n]

        def CMb(name, n=128):
            return cmb[:, coff[name]:coff[name] + n]

        misc = lambda c: cm[:, coff["misc"] + c:coff["misc"] + c + 1]

        gcol = sbt(top, "gcol", [128, 2, 8], F32)
        gcolB = Buf("gcol")
        for l in range(2):
            S_.dma("sp", gcol[:, l, :], attn_norm_d[l].rearrange("(k p) -> p k", p=128), writes=[gcolB],
                   allow_slow_non_contiguous=True)
        gffn = sbt(top, "gffn", [128, 2, D], F32)
        gffnB = Buf("gffn")
        for l in range(2):
            S_.dma("sp", gffn[:, l, :], ffn_norm_d[l].partition_broadcast(128), writes=[gffnB])
        gfin = sbt(top, "gfin", [128, D], F32)
        gfinB = Buf("gfin")
        S_.dma("sp", gfin[:], final_norm_d.partition_broadcast(128), writes=[gfinB])
        wr = sbt(top, "wr", [128, 2, 8, 72], F32)
        wrB = Buf("wr")
        for l in range(2):
            S_.dma("sp", wr[:, l, :, 0:8], moe_w_group_d[l].rearrange("(k p) n -> p k n", p=128), writes=[wrB],
                   allow_slow_non_contiguous=True)
            S_.dma("sp", wr[:, l, :, 8:72], moe_w_expert_d[l].rearrange("(k p) n -> p k n", p=128), writes=[wrB],
                   allow_slow_non_contiguous=True)

        E12 = sbt(top, "E12", [128, 2, NT], F32)
        E12B = Buf("E12")
        R12 = sbt(top, "R12", [128, 2, NT], F32)
        G12 = sbt(top, "G12", [128, 2, NT], F32)
        base = sbt(top, "base", [128, 64], F32)
        R12B, G12B, baseB = Buf("R12"), Buf("G12"), Buf("base")
        DEST = sbt(top, "DEST", [128, 2, NT], I32)
        DESTB = Buf("DEST")

        X1B = [Buf("X1_%d" % t) for t in range(NT)]
        X3B = [Buf("X3_%d" % t) for t in range(NT)]
        H2B = [[Buf("H2_%d_%d" % (l, t)) for t in range(NT + 1)] for l in range(2)]
        H2allB = [Buf("H2all%d" % l) for l in range(2)]
        YtB = [[Buf("Y_%d_%d" % (l, b)) for b in range(NB * SPB)] for l in range(2)]
        YallB = [Buf("Yall%d" % l) for l in range(2)]
        RECB = [Buf("REC%d" % l) for l in range(2)]
        outB = [Buf("out%d" % t) for t in range(NT)]

        def rms_stats(stack_tiles, xt, xtB, n_free, tag):
            junk, junkB, ss, ssB, rstd, rstdB = stack_tiles
            A(lambda e: e.activation(junk[:, 0:n_free], xt, AF.Square, accum_out=ss[:]), [xtB], [junkB, ssB])
            A(lambda e: e.activation(rstd[:], ss[:], AF.Ln, bias=EPS, scale=1.0 / n_free), [ssB], [rstdB])
            A(lambda e: e.activation(rstd[:], rstd[:], AF.Exp, scale=-0.5), [rstdB], [rstdB])

        def moe_pre(l, t, x1t, x1tB, W):
            rms_stats((W["junk"], W["junkB"], W["ss"], W["ssB"], W["rstd"], W["rstdB"]), x1t[:], x1tB, D, "mp")
            h2, h2B = W["h2"], W["h2B"]
            V(lambda e: e.scalar_tensor_tensor(h2[:], x1t[:], W["rstd"][:], gffn[:, l, :], ALU.mult, ALU.mult),
              [x1tB, W["rstdB"], gffnB], [h2B])
            h2b, h2bB = W["h2b"], W["h2bB"]
            G(lambda e: e.tensor_copy(h2b[:].rearrange("t (k p) -> t k p", k=8), h2[:].rearrange("t (p k) -> t k p", k=8)),
              [h2B], [h2bB])
            S_.dma("sp", H2_d[l][t * 128:(t + 1) * 128, :], h2b[:], reads=[h2bB], writes=[H2B[l][t]])
            h2T, h2TB = W["h2T"], W["h2TB"]
            for half in range(2):
                pb, pbB = bank()
                for kk in range(4):
                    kc = half * 4 + kk
                    P(lambda e, kc=kc, kk=kk, pb=pb: e.transpose(pb[:, kk * 128:(kk + 1) * 128], h2[:, kc * 128:(kc + 1) * 128], CM("ident")),
                      [h2B, cmB], [pbB])
                A(lambda e, pb=pb, half=half: e.copy(h2T[:, half * 4:(half + 1) * 4, :], pb[:].rearrange("p (k t) -> p k t", k=4)),
                  [pbB], [h2TB])
            pl, plB = bank()
            for kc in range(8):
                P(lambda e, kc=kc: e.matmul(pl[:, 0:72], h2T[:, kc, :], wr[:, l, kc, :], start=(kc == 0), stop=(kc == 7)),
                  [h2TB, wrB], [plB])
            lg, lgB = W["lg"], W["lgB"]
            V(lambda e: e.tensor_copy(lg[:], pl[:, 0:72]), [plB], [lgB])
            sm, smB = W["sm"], W["smB"]
            t8, t8B = W["t8"], W["t8B"]
            c8, c8B = W["c8"], W["c8B"]
            gm, ngm, gsum, gw, m1, m2, dd, ed, den, g1, g2 = [sm[:, i:i + 1] for i in range(11)]
            V(lambda e: e.tensor_reduce(gm, lg[:, 0:8], AX.X, ALU.max), [lgB], [smB])
            V(lambda e: e.tensor_scalar(c8[:, 0, :], lg[:, 0:8], gm, None, ALU.is_equal), [lgB, smB], [c8B])
            V(lambda e: e.tensor_scalar(ngm, gm, -1.0, None, ALU.mult), [smB], [smB])
            A(lambda e: e.activation(c8[:, 3, :], lg[:, 0:8], AF.Exp, bias=ngm, scale=1.0, accum_out=gsum), [lgB, smB], [c8B, smB])
            V(lambda e: e.reciprocal(gw, gsum), [smB], [smB])
            V(lambda e: e.tensor_tensor(t8[:], lg[:, 8:72].rearrange("p (g e) -> p g e", g=8),
                                        c8[:, 0, :].unsqueeze(2).to_broadcast([128, 8, 8]), ALU.mult), [lgB, c8B], [t8B])
            V(lambda e: e.tensor_reduce(c8[:, 1, :], t8[:].rearrange("p g e -> p e g"), AX.X, ALU.add), [t8B], [c8B])
            V(lambda e: e.tensor_reduce(m1, c8[:, 1, :], AX.X, ALU.max), [c8B], [smB])
            V(lambda e: e.tensor_scalar(c8[:, 2, :], c8[:, 1, :], m1, None, ALU.is_equal), [c8B, smB], [c8B])
            V(lambda e: e.scalar_tensor_tensor(c8[:, 1, :], c8[:, 2, :], -1e30, c8[:, 1, :], ALU.mult, ALU.add), [c8B], [c8B])
            V(lambda e: e.tensor_reduce(m2, c8[:, 1, :], AX.X, ALU.max), [c8B], [smB])
            V(lambda e: e.tensor_scalar(c8[:, 3, :], c8[:, 1, :], m2, None, ALU.is_equal), [c8B, smB], [c8B])
            V(lambda e: e.tensor_tensor(dd, m2, m1, ALU.subtract), [smB], [smB])
            A(lambda e: e.activation(ed, dd, AF.Exp), [smB], [smB])
            V(lambda e: e.tensor_scalar(den, ed, 1.0, None, ALU.add), [smB], [smB])
            V(lambda e: e.reciprocal(den, den), [smB], [smB])
            V(lambda e: e.tensor_tensor(G12[:, 0, t:t + 1], gw, den, ALU.mult), [smB], [G12B])
            V(lambda e: e.tensor_tensor(G12[:, 1, t:t + 1], G12[:, 0, t:t + 1], ed, ALU.mult), [smB, G12B], [G12B])
            oh, ohB = W["oh"], W["ohB"]
            for k, ci in ((0, 2), (1, 3)):
                V(lambda e, k=k, ci=ci: e.tensor_tensor(oh[:, k, :].rearrange("p (g e) -> p g e", g=8),
                                                        c8[:, 0, :].unsqueeze(2).to_broadcast([128, 8, 8]),
                                                        c8[:, ci, :].unsqueeze(1).to_broadcast([128, 8, 8]), ALU.mult),
                  [c8B], [ohB])
            Mb, MbB = W["Mb"], W["MbB"]
            V(lambda e: e.tensor_tensor(Mb[:], oh[:, 0, :], oh[:, 1, :], ALU.add), [ohB], [MbB])
            pr, prB = bank()
            P(lambda e: e.matmul(pr[:, 0:64], CMb("SLT"), Mb[:], start=True, stop=True), [MbB, cmbB], [prB])
            P(lambda e: e.matmul(pr[:, 64:128], CMb("ONES"), Mb[:], start=True, stop=True), [MbB, cmbB], [prB])
            rk, rkB = W["rk"], W["rkB"]
            V(lambda e: e.tensor_tensor(rk[:, 0, :], pr[:, 0:64], base[:], ALU.add), [prB, baseB], [rkB])
            V(lambda e: e.tensor_tensor(base[:], pr[:, 64:128], base[:], ALU.add), [prB, baseB], [baseB])
            for k in range(2):
                V(lambda e, k=k: e.tensor_tensor(rk[:, 1, :], rk[:, 0, :], oh[:, k, :], ALU.mult), [rkB, ohB], [rkB])
                V(lambda e, k=k: e.tensor_reduce(R12[:, k, t:t + 1], rk[:, 1, :], AX.X, ALU.add), [rkB], [R12B])
                V(lambda e, k=k: e.tensor_tensor(rk[:, 1, :], cv[:, 16 + NB:16 + NB + 64], oh[:, k, :], ALU.mult), [cvB, ohB], [rkB])
                V(lambda e, k=k: e.tensor_reduce(E12[:, k, t:t + 1], rk[:, 1, :], AX.X, ALU.add), [rkB], [E12B])

        def moe_pre_tiles(stack):
            W = {}

            def mk(name, shape, dt):
                W[name] = sbt(stack, "mp_" + name, shape, dt)
                W[name + "B"] = Buf("mp_" + name)
            mk("junk", [128, D], F32); mk("ss", [128, 1], F32); mk("rstd", [128, 1], F32)
            mk("h2", [128, D], F32); mk("h2b", [128, D], BF16); mk("h2T", [128, 8, 128], F32)
            mk("lg", [128, 72], F32); mk("sm", [128, 16], F32); mk("t8", [128, 8, 8], F32); mk("c8", [128, 4, 8], F32)
            mk("Mb", [128, 64], BF16); mk("rk", [128, 2, 64], F32); mk("oh", [128, 2, 64], F32)
            return W

        def dispatch(l):
            with contextlib.ExitStack() as st:
                cnt = base
                t16 = sbt(st, "dp_t16", [128, 64, 16], F32); t16B = Buf()
                pad = sbt(st, "dp_pad", [128, 2, 64], F32); padB = Buf()
                V(lambda e: e.tensor_tensor(t16[:], cnt[:].unsqueeze(2).to_broadcast([128, 64, 16]),
                                            cv[:, 0:16].unsqueeze(1).to_broadcast([128, 64, 16]), ALU.is_gt), [baseB, cvB], [t16B])
                V(lambda e: e.tensor_reduce(pad[:, 0, :], t16[:], AX.X, ALU.add), [t16B], [padB])
                V(lambda e: e.tensor_scalar(pad[:, 0, :], pad[:, 0, :], float(BLK), None, ALU.mult), [padB], [padB])
                cs = sbt(st, "dp_cs", [128, 2, 64], F32); csB = Buf()
                V(lambda e: e.tensor_copy(cs[:, 0, :], pad[:, 0, :]), [padB], [csB])
                cur = 0
                sh = 1
                while sh < 64:
                    nxt = 1 - cur
                    V(lambda e, cur=cur, nxt=nxt, sh=sh: e.tensor_copy(cs[:, nxt, 0:sh], cs[:, cur, 0:sh]), [csB], [csB])
                    V(lambda e, cur=cur, nxt=nxt, sh=sh: e.tensor_tensor(cs[:, nxt, sh:64], cs[:, cur, sh:64], cs[:, cur, 0:64 - sh], ALU.add), [csB], [csB])
                    cur = nxt
                    sh *= 2
                pend = cs[:, cur, :]
                V(lambda e: e.tensor_tensor(pad[:, 1, :], pend, pad[:, 0, :], ALU.subtract), [csB, padB], [padB])
                big = sbt(st, "dp_big", [128, NT, 64], F32); bigB = Buf()
                dst = sbt(st, "dp_dst", [128, 2, NT], F32); dstB = Buf()
                for k in range(2):
                    V(lambda e, k=k: e.tensor_tensor(big[:], cv[:, 16 + NB:16 + NB + 64].unsqueeze(1).to_broadcast([128, NT, 64]),
                                                     E12[:, k, :].unsqueeze(2).to_broadcast([128, NT, 64]), ALU.is_equal), [cvB, E12B], [bigB])
                    V(lambda e: e.tensor_tensor(big[:], big[:], pad[:, 1, :].unsqueeze(1).to_broadcast([128, NT, 64]), ALU.mult),
                      [bigB, padB], [bigB])
                    V(lambda e, k=k: e.tensor_reduce(dst[:, k, :], big[:], AX.X, ALU.add), [bigB], [dstB])
                V(lambda e: e.tensor_tensor(dst[:], dst[:], R12[:], ALU.add), [dstB, R12B], [dstB])
                V(lambda e: e.tensor_copy(DEST[:], dst[:]), [dstB], [DESTB])
                be3 = sbt(st, "dp_be3", [128, NB, 64], F32); be3B = Buf()
                bef = sbt(st, "dp_bef", [128, NB], F32); befB = Buf()
                V(lambda e: e.tensor_tensor(be3[:], pend.unsqueeze(1).to_broadcast([128, NB, 64]),
                                            cv[:, 16:16 + NB].unsqueeze(2).to_broadcast([128, NB, 64]), ALU.is_le), [csB, cvB], [be3B])
                V(lambda e: e.tensor_reduce(bef[:], be3[:], AX.X, ALU.add), [be3B], [befB])
                V(lambda e: e.tensor_scalar(bef[:], bef[:], 63.0, None, ALU.min), [befB], [befB])
                wf = sbt(st, "dp_wf", [128, 3, NB], F32); wfB = Buf()
                V(lambda e: e.tensor_scalar(wf[:, 0, :], bef[:], 128.0, misc(2), ALU.mult, ALU.add), [befB, cmB], [wfB])
                V(lambda e: e.tensor_scalar(wf[:, 1, :], bef[:], 256.0, misc(2), ALU.mult, ALU.add), [befB, cmB], [wfB])
                V(lambda e: e.tensor_scalar(wf[:, 2, :], bef[:], 256.0, misc(3), ALU.mult, ALU.add), [befB, cmB], [wfB])
                emp = sbt(st, "dp_emp", [128, NB], F32); empB = Buf()
                V(lambda e: e.tensor_scalar(emp[:], cv[:, 16:16 + NB], cs[:, cur, 63:64], 1.0e5, ALU.is_ge, ALU.mult), [cvB, csB], [empB])
                for k in range(3):
                    V(lambda e, k=k: e.tensor_tensor(wf[:, k, :], wf[:, k, :], emp[:], ALU.add), [wfB, empB], [wfB])
                if l:
                    V(lambda e: e.tensor_scalar(wf[:, 0, :], wf[:, 0, :], float(l * 64 * 128), None, ALU.add), [wfB], [wfB])
                    V(lambda e: e.tensor_scalar(wf[:, 1:3, :], wf[:, 1:3, :], float(l * 64 * 256), None, ALU.add), [wfB], [wfB])
                V(lambda e: e.tensor_copy(WIDX[:], wf[:]), [wfB], [WIDXB])
                rec = sbt(st, "dp_rec", [128, 2, NT, 2], I32); recB = Buf()
                fill = sbt(st, "dp_fill", [128, NSLOT // 128, 2], I32); fillB = Buf()
                V(lambda e: e.memset(fill[:], 0), [], [fillB])
                V(lambda e: e.memset(fill[:, :, 0:1], 1 << 20), [fillB], [fillB])
                S_.dma("sp", REC_d[l].rearrange("(p n) c -> p n c", p=128), fill[:], reads=[fillB], writes=[RECB[l]])
                for k in range(2):
                    V(lambda e, k=k: e.tensor_copy(rec[:, k, :, 0], tokidx[:]), [tokidxB], [recB])
                    V(lambda e, k=k: e.tensor_copy(rec[:].bitcast(F32)[:, k, :, 1], G12[:, k, :]), [G12B], [recB])
                for t in range(NT):
                    for k in range(2):
                        S_.dma("pool", REC_d[l][:, :], rec[:, k, t, :], reads=[recB, DESTB, RECB[l]], writes=[],
                               indirect=dict(out_offset=bass.IndirectOffsetOnAxis(ap=DEST[:, k, t:t + 1], axis=0), in_offset=None))
                S_.fence("pool", [RECB[l]], RECB[l])
                S_.fence("pool", H2B[l], H2allB[l])
                S_.barrier()

        REGS = [nc.gpsimd.to_reg(v) for v in (2 * 64 * 128 - 1, 2 * 64 * 256 - 1, S - 1, NSLOT - 1)]
        WIDX = sbt(top, "WIDX", [128, 3, NB], I32)
        WIDXB = Buf("WIDX")

        def experts(l):
            w1v = moe_w1_d.rearrange("l e (p k) n -> (l e p) (k n)", k=8)
            w3v = moe_w3_d.rearrange("l e (p k) n -> (l e p) (k n)", k=8)
            w2v = moe_w2_d.rearrange("l e f n -> (l e f) n")
            with contextlib.ExitStack() as st:
                NBUF = 5
                PFD = 3
                w1 = [sbt(st, "ex_w1_%d" % i, [128, 8, 256], BF16) for i in range(NBUF)]
                w3 = [sbt(st, "ex_w3_%d" % i, [128, 8, 256], BF16) for i in range(NBUF)]
                w2 = [sbt(st, "ex_w2_%d" % i, [128, 2, D], BF16) for i in range(NBUF)]
                wB = [[Buf(), Buf(), Buf(), Buf()] for i in range(NBUF)]
                rec = [sbt(st, "ex_rec_%d" % i, [128, SPB, 2], I32) for i in range(NBUF)]
                recB = [Buf() for i in range(NBUF)]
                xs = [sbt(st, "ex_xs_%d" % i, [128, SPB, D], BF16) for i in range(NBUF)]
                xsB = [[Buf() for j in range(SPB)] for i in range(NBUF)]
                xsT = [sbt(st, "ex_xsT%d" % i, [128, 8, BLK], BF16) for i in range(2)]; xsTB = [Buf(), Buf()]
                sil = [sbt(st, "ex_sil%d" % i, [128, BLK], BF16) for i in range(2)]; silB = [Buf(), Buf()]
                hid = [sbt(st, "ex_hid%d" % i, [128, 2, BLK], BF16) for i in range(2)]; hidB = [Buf(), Buf()]
                yo = [sbt(st, "ex_yo_%d" % i, [128, D], BF16) for i in range(8)]
                yoB = [[Buf(), Buf()] for i in range(8)]

                yidx = [sbt(st, "ex_yidx_%d" % i, [128, SPB], I32) for i in range(NBUF)]
                yidf = [sbt(st, "ex_yidf_%d" % i, [128, SPB], F32) for i in range(NBUF)]
                yidxB = [Buf() for i in range(NBUF)]
                yidfB = [Buf() for i in range(NBUF)]
                for i in range(NBUF):
                    V(lambda e, i=i: e.memset(xs[i][:], 0.0), [], xsB[i])
                YR0 = 16 + NB + 64
                NW1m, NW2m, Sm, NSLOTm = REGS

                def load(b):
                    i = b % NBUF
                    S_.dma("act", rec[i][:], REC_d[l][b * BLK:(b + 1) * BLK, :].rearrange("(j p) c -> p j c", p=128),
                           reads=[RECB[l]], writes=[recB[i]])
                    S_.dma("pool", w1[i][:].rearrange("p k n -> p (k n)"), w1v, reads=[WIDXB], writes=[wB[i][0]],
                           indirect=dict(out_offset=None, in_offset=bass.IndirectOffsetOnAxis(ap=WIDX[:, 0, b:b + 1], axis=0),
                                         bounds_check=NW1m, oob_is_err=False))
                    S_.dma("pool", w3[i][:].rearrange("p k n -> p (k n)"), w3v, reads=[WIDXB], writes=[wB[i][1]],
                           indirect=dict(out_offset=None, in_offset=bass.IndirectOffsetOnAxis(ap=WIDX[:, 0, b:b + 1], axis=0),
                                         bounds_check=NW1m, oob_is_err=False))
                    for fc in range(2):
                        S_.dma("pool", w2[i][:, fc, :], w2v, reads=[WIDXB], writes=[wB[i][2 + fc]],
                               indirect=dict(out_offset=None, in_offset=bass.IndirectOffsetOnAxis(ap=WIDX[:, 1 + fc, b:b + 1], axis=0),
                                             bounds_check=NW2m, oob_is_err=False))
                    for j in range(SPB):
                        S_.dma("pool", xs[i][:, j, :], H2_d[l][:, :], reads=[recB[i], H2allB[l]], writes=[xsB[i][j]],
                               indirect=dict(out_offset=None, in_offset=bass.IndirectOffsetOnAxis(ap=rec[i][:, j, 0:1], axis=0),
                                             bounds_check=Sm, oob_is_err=False))

                def stT(b):
                    i = b % NBUF
                    x2i = b % 2
                    for j in range(SPB):
                        pb, pbB = bank()
                        pbb = pb[:].bitcast(BF16)
                        for kc in range(8):
                            P(lambda e, kc=kc, j=j, pbb=pbb: e.transpose(pbb[:, kc * 128:(kc + 1) * 128], xs[i][:, j, kc * 128:(kc + 1) * 128], CMb("ident")),
                              [xsB[i][j], cmbB], [pbB])
                        if j % 2 == 0:
                            V(lambda e, j=j, pbb=pbb: e.tensor_copy(xsT[x2i][:, :, j * 128:(j + 1) * 128], pbb.rearrange("p (k t) -> p k t", k=8)),
                              [pbB], [xsTB[x2i]])
                        else:
                            A(lambda e, j=j, pbb=pbb: e.copy(xsT[x2i][:, :, j * 128:(j + 1) * 128], pbb.rearrange("p (k t) -> p k t", k=8)),
                              [pbB], [xsTB[x2i]])

                def stH(b):
                    i = b % NBUF
                    x2i = b % 2
                    for m in range(2):
                        p1, p1B = bank()
                        for kc in range(8):
                            P(lambda e, kc=kc, m=m, p1=p1: e.matmul(p1[:, 0:BLK], w1[i][:, kc, m * 128:(m + 1) * 128], xsT[x2i][:, kc, :], start=(kc == 0), stop=(kc == 7)),
                              [wB[i][0], xsTB[x2i]], [p1B])
                        A(lambda e, m=m, p1=p1: e.activation(sil[m][:], p1[:, 0:BLK], AF.Silu), [p1B], [silB[m]])
                        p3, p3B = bank()
                        for kc in range(8):
                            P(lambda e, kc=kc, m=m, p3=p3: e.matmul(p3[:, 0:BLK], w3[i][:, kc, m * 128:(m + 1) * 128], xsT[x2i][:, kc, :], start=(kc == 0), stop=(kc == 7)),
                              [wB[i][1], xsTB[x2i]], [p3B])
                        V(lambda e, m=m, p3=p3: e.tensor_tensor(hid[x2i][:, m, :], p3[:, 0:BLK], sil[m][:], ALU.mult), [p3B, silB[m]], [hidB[x2i]])

                def stY(b):
                    i = b % NBUF
                    x2i = b % 2
                    for j in range(SPB):
                        yi = (b * SPB + j) % len(yo)
                        for half in range(2):
                            py, pyB = bank()
                            for fc in range(2):
                                P(lambda e, fc=fc, j=j, half=half, py=py: e.matmul(py[:], hid[x2i][:, fc, j * 128:(j + 1) * 128], w2[i][:, fc, half * 512:(half + 1) * 512],
                                                                              start=(fc == 0), stop=(fc == 1)), [hidB[x2i], wB[i][2 + fc]], [pyB])
                            gate = rec[i][:].bitcast(F32)[:, j, 1:2]
                            if half == 0:
                                V(lambda e, py=py, yi=yi, gate=gate: e.tensor_scalar(yo[yi][:, 0:512], py[:], gate, None, ALU.mult), [pyB, recB[i]], [yoB[yi][0]])
                            else:
                                A(lambda e, py=py, yi=yi, gate=gate: e.activation(yo[yi][:, 512:1024], py[:], AF.Copy, scale=gate), [pyB, recB[i]], [yoB[yi][1]])
                        row = (b * SPB + j) * 128
                        S_.dma("sp", Y_d[l][row:row + 128, :], yo[yi][:], reads=[yoB[yi][0], yoB[yi][1]], writes=[YtB[l][b * SPB + j]])

                for b0 in range(min(PFD, NB)):
                    load(b0)
                stT(0)
                for b in range(NB + 1):
                    if b + 1 < NB:
                        stT(b + 1)
                    if b < NB:
                        stH(b)
                    if b - 1 >= 0:
                        stY(b - 1)
                    if b + PFD < NB:
                        load(b + PFD)
                S_.fence("pool", YtB[l], YallB[l])
                S_.barrier()

        def combine(l, t, xold, xoldB, ya, yaB, xo, xoB):
            for k in range(2):
                S_.dma("pool", ya[k][:], Y_d[l][:, :], reads=[DESTB, YallB[l]], writes=[yaB[k]],
                       indirect=dict(out_offset=None, in_offset=bass.IndirectOffsetOnAxis(ap=DEST[:, k, t:t + 1], axis=0)))
            V(lambda e: e.tensor_tensor(xo[:], xold[:], ya[0][:], ALU.add), [xoldB, yaB[0]], [xoB])
            G(lambda e: e.tensor_tensor(xo[:], xo[:], ya[1][:], ALU.add), [xoB, yaB[1]], [xoB])

        def phase_gla():
            with contextlib.ExitStack() as st:
                win = sbt(st, "g_win", [128, 8, 3088], BF16); winB = Buf()
                for kc in range(8):
                    S_.dma("pool", win[:, kc, :], gla_w_in_d[0, kc * 128:(kc + 1) * 128, :], writes=[winB])
                wo = sbt(st, "g_wo", [128, 8, D], BF16); woB = Buf()
                for kc in range(8):
                    S_.dma("pool", wo[:, kc, :], gla_w_o_d[0, kc * 128:(kc + 1) * 128, :], writes=[woB])
                wg = sbt(st, "g_wg", [32, 512], F32); wgB = Buf()
                V(lambda e: e.memset(wg[:], 0.0), [], [wgB])
                S_.dma("sp", wg[0:16, :], gla_w_gate_d[0], writes=[wgB])
                S_.dma("sp", wg[16:17, :], gla_b_gate_d[0:1, :], writes=[wgB])
                onrm = sbt(st, "g_onrm", [128, 256], F32); onrmB = Buf()
                S_.dma("sp", onrm[:], gla_out_norm_d[0].partition_broadcast(128), writes=[onrmB])
                alrT = sbt(st, "g_alrT", [32, 512], F32); alrTB = Buf()
                V(lambda e: e.memset(alrT[:], 1.0), [], [alrTB])
                Sf = [[sbt(st, "g_Sf%d_%d" % (h, i), [128, 256], F32) for i in range(2)] for h in range(4)]
                SfB = [[Buf(), Buf()] for h in range(4)]
                Sb = [[sbt(st, "g_Sb%d_%d" % (h, i), [128, 256], BF16) for i in range(3)] for h in range(4)]
                SbB = [[Buf(), Buf(), Buf()] for h in range(4)]
                for h in range(4):
                    V(lambda e, h=h: e.memset(Sf[h][0][:], 0.0), [], [SfB[h][0]])
                    V(lambda e, h=h: e.memset(Sb[h][0][:], 0.0), [], [SbB[h][0]])
                sf_i = [0] * 4
                sb_i = [0] * 4
                xt = [sbt(st, "g_xt%d" % j, [128, D], F32) for j in range(4)]
                xtB = [Buf() for j in range(4)]
                junk = sbt(st, "g_junk", [128, D], F32); junkB = Buf()
                ss = sbt(st, "g_ss", [128, 1], F32); ssB = Buf()
                rstd = sbt(st, "g_rstd", [128, 1], F32); rstdB = Buf()
                xs = sbt(st, "g_xs", [128, D], BF16); xsB = Buf()
                hT = sbt(st, "g_hT", [128, 8, 512], BF16); hTB = Buf()
                qT = sbt(st, "g_qT", [128, 4, 512], BF16); qTB = Buf()
                kT = sbt(st, "g_kT", [128, 4, 512], BF16); kTB = Buf()
                e1 = sbt(st, "g_e1", [128, 512], F32); e1B = Buf()
                la = sbt(st, "g_la", [128, 512], F32); laB = Buf()
                E = [sbt(st, "g_E%d" % i, [128, 512], F32) for i in range(4)]
                EB = [Buf() for i in range(4)]
                Ed = sbt(st, "g_Ed", [128, 8], F32); EdB = Buf()
                q1T = sbt(st, "g_q1T", [128, 4, 128], BF16); q1TB = Buf()
                k1T = sbt(st, "g_k1T", [128, 4, 128], BF16); k1TB = Buf()
                q2T = sbt(st, "g_q2T", [128, 4, 128], BF16); q2TB = Buf()
                k2 = sbt(st, "g_k2", [128, 512], BF16); k2B = Buf()
                vt = sbt(st, "g_v", [128, D], BF16); vB = Buf()
                gr = sbt(st, "g_gr", [128, D], F32); grB = Buf()
                AmT = sbt(st, "g_AmT", [128, 4, 128], BF16); AmTB = Buf()
                ssq = sbt(st, "g_ssq", [128, 4], F32); ssqB = Buf()
                og = sbt(st, "g_og", [128, D], BF16); ogB = Buf()
                ogT = sbt(st, "g_ogT", [128, 8, 128], BF16); ogTB = Buf()
                x1t = sbt(st, "g_x1t", [128, D], F32); x1tB = Buf()
                W = moe_pre_tiles(st)
                V(lambda e: e.memset(base[:], 0.0), [], [baseB])
                SCALE = 128.0 ** -0.5

                for g in range(NG):
                    for j in range(4):
                        t = g * 4 + j
                        S_.dma("sp", xt[j][:], x_d[t * 128:(t + 1) * 128, :], writes=[xtB[j]])
                    for j in range(4):
                        rms_stats((junk, junkB, ss, ssB, rstd, rstdB), xt[j][:], xtB[j], D, "g")
                        V(lambda e, j=j: e.tensor_scalar(xs[:], xt[j][:], rstd[:], None, ALU.mult), [xtB[j], rstdB], [xsB])
                        pb, pbB = bank()
                        pbb = pb[:].bitcast(BF16)
                        for kc in range(8):
                            P(lambda e, kc=kc, pbb=pbb: e.transpose(pbb[:, kc * 128:(kc + 1) * 128], xs[:, kc * 128:(kc + 1) * 128], CMb("ident")),
                              [xsB, cmbB], [pbB])
                        V(lambda e, j=j, pbb=pbb: e.tensor_tensor(hT[:, :, j * 128:(j + 1) * 128], pbb.rearrange("p (k t) -> p k t", k=8),
                                                                  gcol[:, 0, :].unsqueeze(2).to_broadcast([128, 8, 128]), ALU.mult),
                          [pbB, gcolB], [hTB])
                    for m in range(9):
                        pb, pbB = bank()
                        if m < 8:
                            c0, M = m * 128, 128
                        else:
                            c0, M = 2048, 16
                        for kc in range(8):
                            P(lambda e, kc=kc, c0=c0, M=M, pb=pb: e.matmul(pb[0:M, :], win[:, kc, c0:c0 + M], hT[:, kc, :], start=(kc == 0), stop=(kc == 7)),
                              [winB, hTB], [pbB])
                        if m < 4:
                            A(lambda e, m=m, pb=pb: e.copy(qT[:, m, :], pb[:]), [pbB], [qTB])
                        elif m < 8:
                            V(lambda e, m=m, pb=pb: e.tensor_copy(kT[:, m - 4, :], pb[:]), [pbB], [kTB])
                        else:
                            V(lambda e, pb=pb: e.tensor_copy(alrT[0:16, :], pb[0:16, :]), [pbB], [alrTB])
                    def tile_head(j):
                        t = g * 4 + j
                        js = slice(j * 128, (j + 1) * 128)
                        pz, pzB = bank()
                        P(lambda e, pz=pz, js=js: e.matmul(pz[:], alrT[:, js], wg[:], start=True, stop=True), [alrTB, wgB], [pzB])
                        A(lambda e, pz=pz: e.activation(e1[:], pz[:], AF.Exp, scale=-1.0), [pzB], [e1B])
                        A(lambda e: e.activation(la[:], e1[:], AF.Ln, bias=1.0), [e1B], [laB])
                        pq, pqB = bank()
                        pg, pgB = bank()
                        pk, pkB = bank()
                        pd, pdB = bank()
                        for h in range(4):
                            hs = slice(h * 128, (h + 1) * 128)
                            P(lambda e, hs=hs, pq=pq: e.matmul(pq[:, hs], la[:, hs], CM("TQ"), start=True, stop=True), [laB, cmB], [pqB])
                            P(lambda e, hs=hs, pg=pg: e.matmul(pg[:, hs], la[:, hs], CM("TG"), start=True, stop=True), [laB, cmB], [pgB])
                            P(lambda e, hs=hs, h=h, pd=pd: e.matmul(pd[:, 2 * h:2 * h + 2], la[:, hs], CM("CSG", 2), start=True, stop=True), [laB, cmB], [pdB])
                        P(lambda e, pk=pk: e.matmul(pk[:], CM("TAFT"), la[:], start=True, stop=True), [laB, cmB], [pkB])
                        A(lambda e, pq=pq: e.activation(E[0][:], pq[:], AF.Exp), [pqB], [EB[0]])
                        A(lambda e, pq=pq: e.activation(E[1][:], pq[:], AF.Exp, scale=-1.0), [pqB], [EB[1]])
                        A(lambda e, pg=pg: e.activation(E[2][:], pg[:], AF.Exp), [pgB], [EB[2]])
                        A(lambda e, pk=pk: e.activation(E[3][:], pk[:], AF.Exp), [pkB], [EB[3]])
                        A(lambda e, pd=pd: e.activation(Ed[:], pd[:, 0:8], AF.Exp), [pdB], [EdB])
                        V(lambda e, js=js: e.scalar_tensor_tensor(q1T[:], qT[:, :, js], SCALE, E[0][:].rearrange("p (h t) -> p h t", h=4), ALU.mult, ALU.mult),
                          [qTB, EB[0]], [q1TB])
                        V(lambda e, js=js: e.tensor_tensor(k1T[:], kT[:, :, js], E[1][:].rearrange("p (h t) -> p h t", h=4), ALU.mult), [kTB, EB[1]], [k1TB])
                        V(lambda e, js=js: e.scalar_tensor_tensor(q2T[:], qT[:, :, js], SCALE, E[2][:].rearrange("p (h t) -> p h t", h=4), ALU.mult, ALU.mult),
                          [qTB, EB[2]], [q2TB])
                        pkk, pkkB = bank()
                        for kc in range(8):
                            P(lambda e, kc=kc, pkk=pkk, js=js: e.matmul(pkk[:], hT[:, kc, js], win[:, kc, 512:1024], start=(kc == 0), stop=(kc == 7)), [hTB, winB], [pkkB])
                        V(lambda e, pkk=pkk: e.tensor_tensor(k2[:], pkk[:], E[3][:], ALU.mult), [pkkB, EB[3]], [k2B])
                        for half in range(2):
                            pv, pvB = bank()
                            for kc in range(8):
                                P(lambda e, kc=kc, pv=pv, js=js, half=half: e.matmul(pv[:], hT[:, kc, js], win[:, kc, 1024 + half * 512:1536 + half * 512],
                                                                                 start=(kc == 0), stop=(kc == 7)), [hTB, winB], [pvB])
                            A(lambda e, pv=pv, half=half: e.copy(vt[:, half * 512:(half + 1) * 512], pv[:]), [pvB], [vB])
                        for half in range(2):
                            pr_, prB_ = bank()
                            for kc in range(8):
                                P(lambda e, kc=kc, pr_=pr_, js=js, half=half: e.matmul(pr_[:], hT[:, kc, js], win[:, kc, 2064 + half * 512:2576 + half * 512],
                                                                                   start=(kc == 0), stop=(kc == 7)), [hTB, winB], [prB_])
                            A(lambda e, pr_=pr_, half=half: e.activation(gr[:, half * 512:(half + 1) * 512], pr_[:], AF.Silu), [prB_], [grB])
                        G(lambda e: e.tensor_tensor(gr[:].rearrange("p (h v) -> p h v", h=4), gr[:].rearrange("p (h v) -> p h v", h=4),
                                                    onrm[:].unsqueeze(1).to_broadcast([128, 4, 256]), ALU.mult), [grB, onrmB], [grB])
                        pa, paB = bank()
                        for h in range(4):
                            P(lambda e, h=h, pa=pa: e.matmul(pa[:, h * 128:(h + 1) * 128], k1T[:, h, :], q1T[:, h, :], start=True, stop=True), [k1TB, q1TB], [paB])
                        V(lambda e, pa=pa: e.tensor_tensor(AmT[:], pa[:].rearrange("p (h t) -> p h t", h=4),
                                                           CM("MASK").unsqueeze(1).to_broadcast([128, 4, 128]), ALU.mult), [paB, cmB], [AmTB])

                    def tile_mid(j):
                        t = g * 4 + j
                        js = slice(j * 128, (j + 1) * 128)
                        for hp in range(2):
                            po, poB = bank()
                            for hh in range(2):
                                h = hp * 2 + hh
                                hs = slice(h * 128, (h + 1) * 128)
                                vs = slice(h * 256, (h + 1) * 256)
                                osl = slice(hh * 256, (hh + 1) * 256)
                                sb_prev = sb_i[h]
                                P(lambda e, h=h, vs=vs, osl=osl, po=po: e.matmul(po[0:64, osl], AmT[:, h, 0:64], vt[:, vs], start=True, stop=False), [AmTB, vB], [poB])
                                P(lambda e, h=h, osl=osl, po=po, sb_prev=sb_prev: e.matmul(po[0:64, osl], q2T[:, h, 0:64], Sb[h][sb_prev][:], start=False, stop=True),
                                  [q2TB, SbB[h][sb_prev]], [poB])
                                for c in range(2):
                                    pkv, pkvB = bank()
                                    cs_ = slice(c * 64, (c + 1) * 64)
                                    P(lambda e, cs_=cs_, hs=hs, vs=vs, pkv=pkv: e.matmul(pkv[:, 0:256], k2[cs_, hs], vt[cs_, vs], start=True, stop=True), [k2B, vB], [pkvB])
                                    so, sn = sf_i[h], 1 - sf_i[h]
                                    V(lambda e, h=h, c=c, so=so, sn=sn, pkv=pkv: e.scalar_tensor_tensor(Sf[h][sn][:], Sf[h][so][:], Ed[:, 2 * h + c:2 * h + c + 1], pkv[:, 0:256], ALU.mult, ALU.add),
                                      [SfB[h][so], EdB, pkvB], [SfB[h][sn]])
                                    sf_i[h] = sn
                                    sbn = (sb_i[h] + 1) % 3
                                    G(lambda e, h=h, sn=sn, sbn=sbn: e.tensor_copy(Sb[h][sbn][:], Sf[h][sn][:]), [SfB[h][sn]], [SbB[h][sbn]])
                                    sb_i[h] = sbn
                                    if c == 0:
                                        P(lambda e, h=h, vs=vs, osl=osl, po=po: e.matmul(po[64:128, osl], AmT[:, h, 64:128], vt[:, vs], start=True, stop=False), [AmTB, vB], [poB])
                                        P(lambda e, h=h, osl=osl, po=po, sbn=sbn: e.matmul(po[64:128, osl], q2T[:, h, 64:128], Sb[h][sbn][:], start=False, stop=True),
                                          [q2TB, SbB[h][sbn]], [poB])
                                A(lambda e, h=h, osl=osl, po=po: e.activation(junk[:, 0:256], po[:, osl], AF.Square, accum_out=ssq[:, h:h + 1]), [poB], [junkB, ssqB])
                            A(lambda e, hp=hp: e.activation(ssq[:, 2 * hp:2 * hp + 2], ssq[:, 2 * hp:2 * hp + 2], AF.Ln, bias=EPS, scale=1.0 / 256), [ssqB], [ssqB])
                            A(lambda e, hp=hp: e.activation(ssq[:, 2 * hp:2 * hp + 2], ssq[:, 2 * hp:2 * hp + 2], AF.Exp, scale=-0.5), [ssqB], [ssqB])
                            for hh in range(2):
                                h = hp * 2 + hh
                                vs = slice(h * 256, (h + 1) * 256)
                                osl = slice(hh * 256, (hh + 1) * 256)
                                V(lambda e, h=h, vs=vs, osl=osl, po=po: e.scalar_tensor_tensor(og[:, vs], po[:, osl], ssq[:, h:h + 1], gr[:, vs], ALU.mult, ALU.mult),
                                  [poB, ssqB, grB], [ogB])

                    def tile_tail(j):
                        t = g * 4 + j
                        js = slice(j * 128, (j + 1) * 128)
                        pb, pbB = bank()
                        pbb = pb[:].bitcast(BF16)
                        for kc in range(8):
                            P(lambda e, kc=kc, pbb=pbb: e.transpose(pbb[:, kc * 128:(kc + 1) * 128], og[:, kc * 128:(kc + 1) * 128], CMb("ident")), [ogB, cmbB], [pbB])
                        A(lambda e, pbb=pbb: e.copy(ogT[:], pbb.rearrange("p (k t) -> p k t", k=8)), [pbB], [ogTB])
                        for half in range(2):
                            px, pxB = bank()
                            for kc in range(8):
                                P(lambda e, kc=kc, px=px, half=half: e.matmul(px[:], ogT[:, kc, :], wo[:, kc, half * 512:(half + 1) * 512], start=(kc == 0), stop=(kc == 7)),
                                  [ogTB, woB], [pxB])
                            V(lambda e, px=px, half=half, j=j: e.tensor_tensor(x1t[:, half * 512:(half + 1) * 512], px[:], xt[j][:, half * 512:(half + 1) * 512], ALU.add),
                              [pxB, xtB[j]], [x1tB])
                        S_.dma("sp", X1_d[t * 128:(t + 1) * 128, :], x1t[:], reads=[x1tB], writes=[X1B[t]])
                        moe_pre(0, t, x1t, x1tB, W)

                    tile_head(0)
                    tile_mid(0)
                    for j in range(1, 4):
                        tile_head(j)
                        tile_tail(j - 1)
                        tile_mid(j)
                    tile_tail(3)
                zt = sbt(st, "g_zt", [128, D], BF16); ztB = Buf()
                V(lambda e: e.memset(zt[:], 0.0), [], [ztB])
                for l in range(2):
                    S_.dma("sp", H2_d[l][S:S + 128, :], zt[:], reads=[ztB], writes=[H2B[l][NT]])
                S_.barrier()

        def phase_mla():
            with contextlib.ExitStack() as st:
                win = sbt(st, "m_win", [128, 8, 448 + 64], BF16); winB = Buf()
                for kc in range(8):
                    rows = slice(kc * 128, (kc + 1) * 128)
                    S_.dma("pool", win[:, kc, 0:448], mla_w_in_d[0, rows, :], writes=[winB])
                    S_.dma("pool", win[:, kc, 448:480], mla_w_in_d[0, rows, 416:448], writes=[winB])
                    S_.dma("pool", win[:, kc, 480:512], mla_w_in_d[0, rows, 384:416], writes=[winB])
                wuq = sbt(st, "m_wuq", [128, 2, 8, 256], BF16); wuqB = Buf()
                for qc in range(2):
                    rows = slice(qc * 128, (qc + 1) * 128)
                    S_.dma("pool", wuq[:, qc, :, 0:192], mla_w_uq_d[0, rows, :, :], writes=[wuqB])
                    S_.dma("pool", wuq[:, qc, :, 192:224], mla_w_uq_d[0, rows, :, 160:192], writes=[wuqB])
                    S_.dma("pool", wuq[:, qc, :, 224:256], mla_w_uq_d[0, rows, :, 128:160], writes=[wuqB])
                junk = sbt(st, "m_junk", [128, D], F32); junkB = Buf()
                wuk = junk[:].rearrange("p (h n) -> p h n", h=8); wukB = junkB
                S_.dma("sp", wuk, mla_w_uk_d[0], writes=[wukB])
                wukT = sbt(st, "m_wukT", [128, 8, 128], BF16); wukTB = Buf()
                for hq in range(2):
                    pb, pbB = bank()
                    for hh in range(4):
                        h = hq * 4 + hh
                        P(lambda e, h=h, hh=hh, pb=pb: e.transpose(pb[:, hh * 128:(hh + 1) * 128], wuk[:, h, :], CM("ident")), [wukB, cmB], [pbB])
                    V(lambda e, hq=hq, pb=pb: e.tensor_copy(wukT[:, hq * 4:(hq + 1) * 4, :], pb[:].rearrange("p (h c) -> p h c", h=4)), [pbB], [wukTB])
                wuv = sbt(st, "m_wuv", [128, 8, 128], BF16); wuvB = Buf()
                S_.dma("pool", wuv[:], mla_w_uv_d[0], writes=[wuvB])
                wo = sbt(st, "m_wo", [128, 8, D], BF16); woB = Buf()
                for kc in range(8):
                    S_.dma("pool", wo[:, kc, :], mla_w_o_d[0, kc * 128:(kc + 1) * 128, :], writes=[woB])
                qn = sbt(st, "m_qn", [128, 256], F32); qnB = Buf()
                S_.dma("sp", qn[:], mla_q_norm_d[0].partition_broadcast(128), writes=[qnB])
                kvn = sbt(st, "m_kvn", [128, 128], F32); kvnB = Buf()
                S_.dma("sp", kvn[:], mla_kv_norm_d[0].partition_broadcast(128), writes=[kvnB])
                ckvT = sbt(st, "m_ckvT", [128, S], BF16)
                krT = sbt(st, "m_krT", [64, S], BF16)
                Vaug = sbt(st, "m_Vaug", [128, NT, 132], BF16)
                ckvTB = [Buf() for g in range(NG)]
                krTB = [Buf() for g in range(NG)]
                VaugB = [Buf() for g in range(NG)]
                VaB = Buf()
                V(lambda e: e.memset(Vaug[:], 1.0), [], [VaB])
                for g in range(NG):
                    VaugB[g].w = VaB.w
                posi = sbt(st, "m_posi", [64, 512], I32); posiB = Buf()
                cosT = sbt(st, "m_cos", [64, 512], F32); cosB = Buf()
                sinT = sbt(st, "m_sin", [64, 512], F32); sinB = Buf()
                x2 = [sbt(st, "m_x2_%d" % j, [128, D], F32) for j in range(4)]
                x2B = [Buf() for j in range(4)]
                ya = [sbt(st, "m_ya%d" % k, [128, D], F32) for k in range(2)]
                yaB = [Buf(), Buf()]
                ss = sbt(st, "m_ss", [128, 1], F32); ssB = Buf()
                rstd = sbt(st, "m_rstd", [128, 1], F32); rstdB = Buf()
                xs = sbt(st, "m_xs", [128, D], BF16); xsB = Buf()
                hT = sbt(st, "m_hT", [128, 8, 512], BF16); hTB = Buf()
                cq = sbt(st, "m_cq", [128, 256], BF16); cqB = Buf()
                cqT = sbt(st, "m_cqT", [128, 2, 512], BF16); cqTB = Buf()
                ckv = sbt(st, "m_ckv", [128, 128], BF16); ckvB = Buf()
                tmpf = sbt(st, "m_tmpf", [128, 512], F32); tmpfB = Buf()
                tmp2 = sbt(st, "m_tmp2", [128, 512], F32); tmp2B = Buf()
                ang, angB, ang2, ang2B = tmpf[0:64, :], tmpfB, tmp2[0:64, :], tmp2B
                qnT = sbt(st, "m_qnT", [128, 2, 512], BF16); qnTB = [Buf(), Buf()]
                qlatT = sbt(st, "m_qlatT", [128, 2, 512], BF16); qlatTB = [Buf(), Buf()]
                qrT = sbt(st, "m_qrT", [64, 2, 512], BF16); qrTB = [Buf(), Buf()]
                PT = [sbt(st, "m_PT%d" % i, [128, 512], BF16) for i in range(5)]
                PTB = [Buf() for i in range(5)]
                rec_ = sbt(st, "m_recip", [128, 4], F32); rec_B = Buf()
                olat = sbt(st, "m_olat", [128, 4, 128], BF16); olatB = Buf()
                olT = sbt(st, "m_olT", [128, 512], BF16); olTB = Buf()
                oT = sbt(st, "m_oT", [128, 8, 512], BF16); oTB = Buf()
                x3t = sbt(st, "m_x3t", [128, D], F32); x3tB = Buf()
                W = moe_pre_tiles(st)
                print("MLA phase sbuf bytes remaining", nc.sbuf_bytes_remaining, flush=True)
                V(lambda e: e.memset(base[:], 0.0), [], [baseB])
                MSCALE = 192.0 ** -0.5
                TWO_PI = 2.0 * math.pi
                pt_i = [0]

                for g in range(NG):
                    gs = slice(g * 512, (g + 1) * 512)
                    S_.dma("sp", posi[:], pos_d[g * 512:(g + 1) * 512].partition_broadcast(64), writes=[posiB])
                    V(lambda e: e.tensor_copy(ang[:], posi[:]), [posiB], [angB])
                    V(lambda e: e.tensor_scalar(ang[:], ang[:], cm[0:64, coff["misc"]:coff["misc"] + 1], None, ALU.mult), [angB, cmB], [angB])
                    V(lambda e: e.tensor_scalar(ang2[:], ang[:], math.pi / 2, None, ALU.add), [angB], [ang2B])
                    MAGIC = 12582912.0
                    PIS = 3.1415925
                    for (dst, dstB, src, srcB) in ((sinT, sinB, ang, angB), (cosT, cosB, ang2, ang2B)):
                        V(lambda e, dst=dst, src=src: e.tensor_scalar(dst[:], src[:], 1.0 / TWO_PI, MAGIC, ALU.mult, ALU.add), [srcB], [dstB])
                        V(lambda e, dst=dst: e.tensor_scalar(dst[:], dst[:], -MAGIC, None, ALU.add), [dstB], [dstB])
                        V(lambda e, dst=dst, src=src: e.scalar_tensor_tensor(dst[:], dst[:], -TWO_PI, src[:], ALU.mult, ALU.add), [dstB, srcB], [dstB])
                        V(lambda e, dst=dst: e.tensor_scalar(dst[:], dst[:], -PIS, PIS, ALU.max, ALU.min), [dstB], [dstB])
                        A(lambda e, dst=dst: e.activation(dst[:], dst[:], AF.Sin), [dstB], [dstB])
                    V(lambda e: e.tensor_scalar(sinT[:], sinT[:], cm[0:64, coff["misc"] + 1:coff["misc"] + 2], None, ALU.mult), [sinB, cmB], [sinB])
                    for j in range(4):
                        t = g * 4 + j
                        S_.dma("sp", x2[j][:], X1_d[t * 128:(t + 1) * 128, :], reads=[X1B[t]], writes=[x2B[j]])
                    for j in range(4):
                        t = g * 4 + j
                        js = slice(j * 128, (j + 1) * 128)
                        combine(0, t, x2[j], x2B[j], ya, yaB, x2[j], x2B[j])
                        rms_stats((junk, junkB, ss, ssB, rstd, rstdB), x2[j][:], x2B[j], D, "m")
                        V(lambda e, j=j: e.tensor_scalar(xs[:], x2[j][:], rstd[:], None, ALU.mult), [x2B[j], rstdB], [xsB])
                        pb, pbB = bank()
                        pbb = pb[:].bitcast(BF16)
                        for kc in range(8):
                            P(lambda e, kc=kc, pbb=pbb: e.transpose(pbb[:, kc * 128:(kc + 1) * 128], xs[:, kc * 128:(kc + 1) * 128], CMb("ident")), [xsB, cmbB], [pbB])
                        V(lambda e, js=js, pbb=pbb: e.tensor_tensor(hT[:, :, js], pbb.rearrange("p (k t) -> p k t", k=8),
                                                                   gcol[:, 1, :].unsqueeze(2).to_broadcast([128, 8, 128]), ALU.mult), [pbB, gcolB], [hTB])
                        pp, ppB = bank()
                        for kc in range(8):
                            P(lambda e, kc=kc, pp=pp, js=js: e.matmul(pp[:, 0:384], hT[:, kc, js], win[:, kc, 0:384], start=(kc == 0), stop=(kc == 7)), [hTB, winB], [ppB])
                        A(lambda e, pp=pp: e.activation(junk[:, 0:256], pp[:, 0:256], AF.Square, accum_out=ss[:]), [ppB], [junkB, ssB])
                        A(lambda e: e.activation(rstd[:], ss[:], AF.Ln, bias=EPS, scale=1.0 / 256), [ssB], [rstdB])
                        A(lambda e: e.activation(rstd[:], rstd[:], AF.Exp, scale=-0.5), [rstdB], [rstdB])
                        V(lambda e, pp=pp: e.scalar_tensor_tensor(cq[:], pp[:, 0:256], rstd[:], qn[:], ALU.mult, ALU.mult), [ppB, rstdB, qnB], [cqB])
                        A(lambda e, pp=pp: e.activation(junk[:, 0:128], pp[:, 256:384], AF.Square, accum_out=ss[:]), [ppB], [junkB, ssB])
                        A(lambda e: e.activation(rstd[:], ss[:], AF.Ln, bias=EPS, scale=1.0 / 128), [ssB], [rstdB])
                        A(lambda e: e.activation(rstd[:], rstd[:], AF.Exp, scale=-0.5), [rstdB], [rstdB])
                        V(lambda e, pp=pp, t=t: e.scalar_tensor_tensor(Vaug[:, t, 0:128], pp[:, 256:384], rstd[:], kvn[:], ALU.mult, ALU.mult), [ppB, rstdB, kvnB], [VaugB[g]])
                        pb, pbB = bank()
                        pbb = pb[:].bitcast(BF16)
                        for qc in range(2):
                            P(lambda e, qc=qc, pbb=pbb: e.transpose(pbb[:, qc * 128:(qc + 1) * 128], cq[:, qc * 128:(qc + 1) * 128], CMb("ident")), [cqB, cmbB], [pbB])
                        P(lambda e, pbb=pbb, t=t: e.transpose(pbb[:, 256:384], Vaug[:, t, 0:128], CMb("ident")), [VaugB[g], cmbB], [pbB])
                        V(lambda e, js=js, pbb=pbb: e.tensor_copy(cqT[:, :, js], pbb[:, 0:256].rearrange("p (q t) -> p q t", q=2)), [pbB], [cqTB])
                        V(lambda e, t=t, pbb=pbb: e.tensor_copy(ckvT[:, t * 128:(t + 1) * 128], pbb[:, 256:384]), [pbB], [ckvTB[g]])
                    pk1, pk1B = bank()
                    pk2, pk2B = bank()
                    for kc in range(8):
                        P(lambda e, kc=kc, pk1=pk1: e.matmul(pk1[0:64, :], win[:, kc, 384:448], hT[:, kc, :], start=(kc == 0), stop=(kc == 7)), [winB, hTB], [pk1B])
                    for kc in range(8):
                        P(lambda e, kc=kc, pk2=pk2: e.matmul(pk2[0:64, :], win[:, kc, 448:512], hT[:, kc, :], start=(kc == 0), stop=(kc == 7)), [winB, hTB], [pk2B])
                    V(lambda e, pk2=pk2: e.tensor_tensor(tmpf[0:64, :], pk2[0:64, :], sinT[:], ALU.mult), [pk2B, sinB], [tmpfB])
                    V(lambda e, pk1=pk1: e.tensor_tensor(tmp2[0:64, :], pk1[0:64, :], cosT[:], ALU.mult), [pk1B, cosB], [tmp2B])
                    V(lambda e, gs=gs: e.tensor_tensor(krT[:, gs], tmpf[0:64, :], tmp2[0:64, :], ALU.add), [tmpfB, tmp2B], [krTB[g]])
                    nkb = 4 * g + 4
                    ACC = (0, 1, 2, 3)
                    qst = {}

                    def qproj1(h):
                        hb = h % 2
                        pn, pnB = bank(ACC)
                        for qc in range(2):
                            P(lambda e, qc=qc: e.matmul(pn[:], wuq[:, qc, h, 0:128], cqT[:, qc, :], start=(qc == 0), stop=(qc == 1)), [wuqB, cqTB], [pnB])
                        A(lambda e: e.copy(qnT[:, hb, :], pn[:]), [pnB], [qnTB[hb]])
                        pr1, pr1B = bank(ACC)
                        for qc in range(2):
                            P(lambda e, qc=qc: e.matmul(pr1[0:64, :], wuq[:, qc, h, 128:192], cqT[:, qc, :], start=(qc == 0), stop=(qc == 1)), [wuqB, cqTB], [pr1B])
                        V(lambda e: e.scalar_tensor_tensor(tmp2[0:64, :], pr1[0:64, :], MSCALE, cosT[:], ALU.mult, ALU.mult), [pr1B, cosB], [tmp2B])
                        pr2, pr2B = bank(ACC)
                        for qc in range(2):
                            P(lambda e, qc=qc: e.matmul(pr2[0:64, :], wuq[:, qc, h, 192:256], cqT[:, qc, :], start=(qc == 0), stop=(qc == 1)), [wuqB, cqTB], [pr2B])
                        V(lambda e: e.scalar_tensor_tensor(tmpf[0:64, :], pr2[0:64, :], MSCALE, sinT[:], ALU.mult, ALU.mult), [pr2B, sinB], [tmpfB])
                        V(lambda e: e.tensor_tensor(qrT[:, hb, :], tmpf[0:64, :], tmp2[0:64, :], ALU.add), [tmpfB, tmp2B], [qrTB[hb]])

                    def qproj2(h):
                        hb = h % 2
                        pl_, plB_ = bank(ACC)
                        P(lambda e: e.matmul(pl_[:], wukT[:, h, :], qnT[:, hb, :], start=True, stop=True), [wukTB, qnTB[hb]], [plB_])
                        A(lambda e: e.activation(qlatT[:, hb, :], pl_[:], AF.Copy, scale=MSCALE), [plB_], [qlatTB[hb]])

                    st_psc = {}
                    st_pi = {}

                    def stA(h, kb):
                        hb = h % 2
                        kg = kb // 4
                        c0 = max(kb - 4 * g, 0) * 128
                        psc, pscB = bank(ACC)
                        st_psc[(h, kb)] = (psc, pscB)
                        ks = slice(kb * 128, (kb + 1) * 128)
                        P(lambda e: e.matmul(psc[:, c0:512], ckvT[:, ks], qlatT[:, hb, c0:512], start=True, stop=False),
                          [ckvTB[kg], qlatTB[hb]], [pscB])
                        P(lambda e: e.matmul(psc[:, c0:512], krT[:, ks], qrT[:, hb, c0:512], start=False, stop=True),
                          [krTB[kg], qrTB[hb]], [pscB])

                    def stB(h, kb):
                        jd = kb - 4 * g
                        c0 = max(jd, 0) * 128
                        psc, pscB = st_psc.pop((h, kb))
                        pi = pt_i[0] % len(PT)
                        pt_i[0] += 1
                        st_pi[(h, kb)] = pi
                        A(lambda e: e.activation(PT[pi][:, c0:512], psc[:, c0:512], AF.Exp), [pscB], [PTB[pi]])
                        if jd >= 0:
                            G(lambda e: e.tensor_tensor(PT[pi][:, c0:c0 + 128], PT[pi][:, c0:c0 + 128], CMb("TRI"), ALU.mult), [PTB[pi], cmbB], [PTB[pi]])

                    def stC(h, kb):
                        kg = kb // 4
                        jd = kb - 4 * g
                        pi = st_pi.pop((h, kb))
                        for i in range(max(jd, 0), 4):
                            last = (kb == 4 * g + i)
                            P(lambda e, i=i, last=last: e.matmul(ps[i][:, 0:129], PT[pi][:, i * 128:(i + 1) * 128], Vaug[:, kb, 0:129],
                                                                 start=(kb == 0), stop=last), [PTB[pi], VaugB[kg]], [psB[i]])
                        if kb == nkb - 1:
                            stF(h)

                    def stF(h):
                        for i in range(4):
                            V(lambda e, i=i: e.reciprocal(rec_[:, i:i + 1], ps[i][:, 128:129]), [psB[i]], [rec_B])
                            V(lambda e, i=i: e.tensor_scalar(olat[:, i, :], ps[i][:, 0:128], rec_[:, i:i + 1], None, ALU.mult), [psB[i], rec_B], [olatB])
                        pb, pbB = bank(ACC)
                        pbb = pb[:].bitcast(BF16)
                        for i in range(4):
                            P(lambda e, i=i: e.transpose(pbb[:, i * 128:(i + 1) * 128], olat[:, i, :], CMb("ident")), [olatB, cmbB], [pbB])
                        V(lambda e: e.tensor_copy(olT[:], pbb[:, 0:512]), [pbB], [olTB])
                        pv_, pvB_ = bank(ACC)
                        P(lambda e: e.matmul(pv_[:], wuv[:, h, :], olT[:], start=True, stop=True), [wuvB, olTB], [pvB_])
                        A(lambda e: e.copy(oT[:, h, :], pv_[:]), [pvB_], [oTB])

                    steps = [(h, kb) for h in range(8) for kb in range(nkb)]
                    qproj1(0)
                    qproj2(0)
                    LA_B, LA_C = 1, 3
                    for n in range(len(steps) + LA_C):
                        if 0 <= n - LA_B < len(steps):
                            stB(*steps[n - LA_B])
                        if n < len(steps):
                            h, kb = steps[n]
                            if kb == 0 and h + 1 < 8:
                                qproj1(h + 1)
                            if kb == 2 and h + 1 < 8:
                                qproj2(h + 1)
                            stA(h, kb)
                        if 0 <= n - LA_C < len(steps):
                            stC(*steps[n - LA_C])
                    for j in range(4):
                        t = g * 4 + j
                        js = slice(j * 128, (j + 1) * 128)
                        for half in range(2):
                            px, pxB = bank()
                            for h in range(8):
                                P(lambda e, h=h, px=px, half=half, js=js: e.matmul(px[:], oT[:, h, js], wo[:, h, half * 512:(half + 1) * 512], start=(h == 0), stop=(h == 7)),
                                  [oTB, woB], [pxB])
                            V(lambda e, px=px, half=half, j=j: e.tensor_tensor(x3t[:, half * 512:(half + 1) * 512], px[:], x2[j][:, half * 512:(half + 1) * 512], ALU.add),
                              [pxB, x2B[j]], [x3tB])
                        S_.dma("sp", X3_d[t * 128:(t + 1) * 128, :], x3t[:], reads=[x3tB], writes=[X3B[t]])
                        moe_pre(1, t, x3t, x3tB, W)
                S_.barrier()

        def phase_final():
            with contextlib.ExitStack() as st:
                xold = [sbt(st, "f_xold%d" % i, [128, D], F32) for i in range(2)]
                xoldB = [Buf(), Buf()]
                ya = [[sbt(st, "f_ya%d_%d" % (i, k), [128, D], F32) for k in range(2)] for i in range(2)]
                yaB = [[Buf(), Buf()] for i in range(2)]
                x4 = [sbt(st, "f_x4_%d" % i, [128, D], F32) for i in range(2)]; x4B = [Buf(), Buf()]
                junk = [sbt(st, "f_junk%d" % i, [128, D], F32) for i in range(2)]; junkB = [Buf(), Buf()]
                ss = [sbt(st, "f_ss%d" % i, [128, 1], F32) for i in range(2)]; ssB = [Buf(), Buf()]
                rstd = [sbt(st, "f_rstd%d" % i, [128, 1], F32) for i in range(2)]; rstdB = [Buf(), Buf()]
                yo = [sbt(st, "f_yo%d" % i, [128, D], F32) for i in range(2)]
                yoB = [Buf(), Buf()]

                def s1(t):
                    i = t % 2
                    S_.dma("sp", xold[i][:], X3_d[t * 128:(t + 1) * 128, :], reads=[X3B[t]], writes=[xoldB[i]])
                    combine(1, t, xold[i], xoldB[i], ya[i], yaB[i], x4[i], x4B[i])
                    rms_stats((junk[i], junkB[i], ss[i], ssB[i], rstd[i], rstdB[i]), x4[i][:], x4B[i], D, "f")

                def s2(t):
                    i = t % 2
                    V(lambda e: e.scalar_tensor_tensor(yo[i][:], x4[i][:], rstd[i][:], gfin[:], ALU.mult, ALU.mult), [x4B[i], rstdB[i], gfinB], [yoB[i]])
                    S_.dma("act", out_d[t * 128:(t + 1) * 128, :], yo[i][:], reads=[yoB[i]], writes=[outB[t]])

                s1(0)
                for t in range(NT):
                    if t + 1 < NT:
                        s1(t + 1)
                    s2(t)

        phase_gla()
        dispatch(0)
        experts(0)
        phase_mla()
        dispatch(1)
        experts(1)
        phase_final()
        S_.finish()
        print("bass program: instructions", S_.n_ins, "waits", S_.n_wait, flush=True)
    return nc, (cmat_np, cvec_np, tokidx_np)


_CACHE = {}


def kernel(**inputs):
    x = np.asarray(inputs["x"])
    B, S, _ = x.shape
    BLK = 512 if S >= 2048 else 128
    key = (S, BLK)
    if key not in _CACHE:
        _CACHE[key] = build_nc(S, BLK)
    nc, (cmat, cvec, tokidx) = _CACHE[key]
    names = ["attn_norm", "ffn_norm", "final_norm", "gla_w_in", "gla_w_gate", "gla_b_gate", "gla_out_norm", "gla_w_o",
             "mla_w_in", "mla_q_norm", "mla_w_uq", "mla_kv_norm", "mla_w_uk", "mla_w_uv", "mla_w_o",
             "moe_w_group", "moe_w_expert", "moe_w1", "moe_w3", "moe_w2"]
    shared = {n: np.ascontiguousarray(np.asarray(inputs[n], dtype=np.float32)) for n in names}
    shared["cmat"] = cmat
    shared["cvec"] = cvec
    shared["tokidx"] = tokidx
    pos = np.asarray(inputs["positions"]).astype(np.int32)
    in_maps = []
    for b in range(B):
        m = dict(shared)
        m["x"] = np.ascontiguousarray(x[b], dtype=np.float32)
        m["positions"] = np.ascontiguousarray(pos[b])
        in_maps.append(m)
    res = run_bass_kernel_spmd(nc, in_maps, core_ids=list(range(B)))
    out = np.stack([np.asarray(r["out"], dtype=np.float32) for r in res.results], axis=0)
    return out
```

```python
import contextlib
import math
import numpy as np
import concourse.bass as bass
import concourse.mybir as mybir
from concourse.bass_utils import run_bass_kernel_spmd

F32 = mybir.dt.float32
BF16 = mybir.dt.bfloat16
I32 = mybir.dt.int32
ALU = mybir.AluOpType
AF = mybir.ActivationFunctionType
AX = mybir.AxisListType

SAME_ENGINE_SYNC = "raw"
D = 1024
EPS = 1e-6
NE = 64


class Buf:
    __slots__ = ("name", "w", "r")

    def __init__(self, name=""):
        self.name = name
        self.w = None
        self.r = {}


class Sched:
    ENG_NAMES = ("pe", "act", "dve", "pool", "sp")

    def __init__(self, nc, stack, n_dma=(16, 12, 6)):
        self.nc = nc
        self.engs = {"pe": nc.tensor, "act": nc.scalar, "dve": nc.vector,
                     "pool": nc.gpsimd, "sp": nc.sync}
        self.sems = {}
        self.cnt = {}
        for e in self.ENG_NAMES:
            self.sems[e] = stack.enter_context(nc.semaphore("s_" + e))
            self.cnt[e] = 0
        self.dma_pool = {}
        self.dma_rr = {}
        for q, n in zip(("sp", "pool", "act"), n_dma):
            keys = []
            for i in range(n):
                k = "d_%s_%d" % (q, i)
                self.sems[k] = stack.enter_context(nc.semaphore(k))
                self.cnt[k] = 0
                keys.append(k)
            self.dma_pool[q] = keys
            self.dma_rr[q] = 0
        self.seen = {e: {} for e in self.ENG_NAMES}
        self.n_wait = 0
        self.n_ins = 0

    def _wait(self, eng, tickets):
        need = {}
        seen = self.seen[eng]
        for t in tickets:
            if t is None:
                continue
            s, v = t[0], t[1]
            if s == eng and (eng == "pe" or not SAME_ENGINE_SYNC or (SAME_ENGINE_SYNC == "raw" and len(t) > 2)):
                continue
            if seen.get(s, 0) >= v:
                continue
            if need.get(s, 0) < v:
                need[s] = v
        for s, v in need.items():
            self.engs[eng].wait_ge(self.sems[s], v)
            seen[s] = v
            self.n_wait += 1

    def _deps(self, reads, writes):
        tk = []
        for b in reads:
            tk.append(b.w)
        for b in writes:
            if b.w is not None:
                tk.append((b.w[0], b.w[1], "waw"))
            for s, v in b.r.items():
                tk.append((s, v, "war"))
        return tk

    def _commit(self, tk, reads, writes):
        s, v = tk
        for b in reads:
            if b.r.get(s, 0) < v:
                b.r[s] = v
        for b in writes:
            b.w = tk
            b.r = {}

    def op(self, eng, fn, reads=(), writes=()):
        self._wait(eng, self._deps(reads, writes))
        ins = fn(self.engs[eng])
        self.cnt[eng] += 1
        ins.then_inc(self.sems[eng], 1)
        tk = (eng, self.cnt[eng])
        self._commit(tk, reads, writes)
        self.n_ins += 1
        return tk

    def dma(self, q, out, in_, reads=(), writes=(), indirect=None, **kw):
        keys = self.dma_pool[q]
        k = keys[self.dma_rr[q] % len(keys)]
        self.dma_rr[q] += 1
        deps = self._deps(reads, writes)
        if self.cnt[k]:
            deps.append((k, self.cnt[k]))
        self._wait(q, deps)
        if indirect is None:
            ins = self.engs[q].dma_start(out=out, in_=in_, **kw)
        else:
            ins = self.engs[q].indirect_dma_start(out=out, in_=in_, **indirect)
        self.cnt[k] += 16
        ins.then_inc(self.sems[k], 16)
        tk = (k, self.cnt[k])
        self._commit(tk, reads, writes)
        self.n_ins += 1
        return tk

    def all_tickets(self):
        return [(k, v) for k, v in self.cnt.items() if v]

    def barrier(self):
        tk = self.all_tickets()
        for e in self.ENG_NAMES:
            self._wait(e, tk)

    def fence(self, eng, bufs_in, buf_out):
        tk = []
        for b in bufs_in:
            tk.append(b.w)
            for s, v in b.r.items():
                tk.append((s, v))
        self._wait(eng, tk)
        return self.op(eng, lambda e: e.memset(self.fence_t[:], 0.0), writes=[buf_out, self.fence_b])

    def finish(self):
        self._wait("sp", self.all_tickets())


def make_consts(S, BLK):
    NT = S // 128
    NSLOT = 2 * S + NE * BLK
    NB = NSLOT // BLK
    j = np.arange(128)[:, None]
    i = np.arange(128)[None, :]
    same = (j // 64) == (i // 64)
    Tinc = ((j <= i) & same).astype(np.float32)
    mid = (i // 64) * 64 + 32
    Tmid = ((j <= mid) & same).astype(np.float32)
    mats = {
        "ident": np.eye(128, dtype=np.float32),
        "TG": -Tinc / 16.0,
        "TQ": -(Tinc - Tmid) / 16.0,
        "TAFT": -((j > i) & same).astype(np.float32) / 16.0,
        "MASK": Tinc,
        "TRI": (j <= i).astype(np.float32),
        "SLT": (j < i).astype(np.float32),
        "ONES": np.ones((128, 128), np.float32),
    }
    cs = np.zeros((128, 128), np.float32)
    cs[:64, 0] = -1.0 / 16.0
    cs[64:, 1] = -1.0 / 16.0
    mats["CSG"] = cs
    misc = np.zeros((128, 128), np.float32)
    p = np.arange(128)
    inv_freq = 1.0 / (10000.0 ** (np.arange(0, 64, 2, dtype=np.float32) / 64.0))
    misc[:, 0] = inv_freq.astype(np.float32)[p % 32]
    misc[:, 1] = np.where((p % 64) < 32, -1.0, 1.0)
    misc[:, 2] = p
    misc[:, 3] = p + 128
    mats["misc"] = misc
    names = list(mats.keys())
    cmat = np.concatenate([mats[n] for n in names], axis=1).astype(np.float32)
    off = {n: k * 128 for k, n in enumerate(names)}
    thr = np.broadcast_to((np.arange(16, dtype=np.float32) * BLK)[None, :], (128, 16))
    tblk = np.broadcast_to((np.arange(NB, dtype=np.float32) * BLK)[None, :], (128, NB))
    iota = np.broadcast_to(np.arange(64, dtype=np.float32)[None, :], (128, 64))
    SPB = BLK // 128
    yrow = (np.arange(NB * SPB, dtype=np.float32)[None, :] * 128 + np.arange(128, dtype=np.float32)[:, None])
    cvec = np.concatenate([thr, tblk, iota, yrow], axis=1).astype(np.float32).copy()
    tokidx = (np.arange(NT, dtype=np.int32)[None, :] * 128 + np.arange(128, dtype=np.int32)[:, None]).astype(np.int32)
    return cmat, off, cvec, tokidx


def build_nc(S, BLK, dbg=None):
    NT = S // 128
    NG = S // 512
    NSLOT = 2 * S + NE * BLK
    NB = NSLOT // BLK
    SPB = BLK // 128
    cmat_np, coff, cvec_np, tokidx_np = make_consts(S, BLK)
    NCM = cmat_np.shape[1]
    NCV = cvec_np.shape[1]

    nc = bass.Bass("TRN2", target_bir_lowering=False)

    def din(name, shape, dt=F32):
        return nc.dram_tensor(name, list(shape), dt, kind="ExternalInput").ap()

    x_d = din("x", [S, D])
    pos_d = din("positions", [S], I32)
    attn_norm_d = din("attn_norm", [2, D])
    ffn_norm_d = din("ffn_norm", [2, D])
    final_norm_d = din("final_norm", [D])
    gla_w_in_d = din("gla_w_in", [1, D, 3088])
    gla_w_gate_d = din("gla_w_gate", [1, 16, 512])
    gla_b_gate_d = din("gla_b_gate", [1, 512])
    gla_out_norm_d = din("gla_out_norm", [1, 256])
    gla_w_o_d = din("gla_w_o", [1, D, D])
    mla_w_in_d = din("mla_w_in", [1, D, 448])
    mla_q_norm_d = din("mla_q_norm", [1, 256])
    mla_w_uq_d = din("mla_w_uq", [1, 256, 8, 192])
    mla_kv_norm_d = din("mla_kv_norm", [1, 128])
    mla_w_uk_d = din("mla_w_uk", [1, 128, 8, 128])
    mla_w_uv_d = din("mla_w_uv", [1, 128, 8, 128])
    mla_w_o_d = din("mla_w_o", [1, D, D])
    moe_w_group_d = din("moe_w_group", [2, D, 8])
    moe_w_expert_d = din("moe_w_expert", [2, D, 64])
    moe_w1_d = din("moe_w1", [2, 64, D, 256])
    moe_w3_d = din("moe_w3", [2, 64, D, 256])
    moe_w2_d = din("moe_w2", [2, 64, 256, D])
    cmat_d = din("cmat", [128, NCM])
    cvec_d = din("cvec", [128, NCV])
    tokidx_d = din("tokidx", [128, NT], I32)
    out_d = nc.dram_tensor("out", [S, D], F32, kind="ExternalOutput").ap()

    def dscr(name, shape, dt=F32):
        return nc.dram_tensor(name, list(shape), dt, kind="Internal").ap()

    X1_d = dscr("X1s", [S, D])
    X3_d = dscr("X3s", [S, D])
    H2_d = [dscr("H2s%d" % l, [S + 128, D], BF16) for l in range(2)]
    Y_d = [dscr("Ys%d" % l, [NSLOT, D], BF16) for l in range(2)]
    REC_d = [dscr("RECs%d" % l, [NSLOT, 2], I32) for l in range(2)]

    top = contextlib.ExitStack()
    with top:
        S_ = Sched(nc, top)

        uniq = [0]

        def sbt(stack, name, shape, dt):
            uniq[0] += 1
            return stack.enter_context(nc.sbuf_tensor("%s_u%d" % (name, uniq[0]), list(shape), dt))

        S_.fence_t = sbt(top, "fence_t", [128, 1], F32)
        S_.fence_b = Buf("fence")

        def V(fn, r=(), w=()):
            return S_.op("dve", fn, r, w)

        def A(fn, r=(), w=()):
            return S_.op("act", fn, r, w)

        def P(fn, r=(), w=()):
            return S_.op("pe", fn, r, w)

        def G(fn, r=(), w=()):
            return S_.op("pool", fn, r, w)

        ps = [top.enter_context(nc.psum_tensor("ps%d" % i, [128, 512], F32)) for i in range(8)]
        psB = [Buf("ps%d" % i) for i in range(8)]
        ps_rr = [0]

        def bank(avoid=()):
            while True:
                i = ps_rr[0] % 8
                ps_rr[0] += 1
                if i not in avoid:
                    return ps[i], psB[i]

        cm = sbt(top, "cm", [128, NCM], F32)
        cmB = Buf("cm")
        S_.dma("sp", cm[:], cmat_d, writes=[cmB])
        cmb = sbt(top, "cmb", [128, NCM], BF16)
        cmbB = Buf("cmb")
        V(lambda e: e.tensor_copy(cmb[:], cm[:]), [cmB], [cmbB])
        cv = sbt(top, "cv", [128, NCV], F32)
        cvB = Buf("cv")
        S_.dma("sp", cv[:], cvec_d, writes=[cvB])
        tokidx = sbt(top, "tokidx_sb", [128, NT], I32)
        tokidxB = Buf("tokidx")
        S_.dma("sp", tokidx[:], tokidx_d, writes=[tokidxB])

        def CM(name, n=128):
            return cm[:, coff[name]:coff[name] + n]

        def CMb(name, n=128):
            return cmb[:, coff[name]:coff[name] + n]

        misc = lambda c: cm[:, coff["misc"] + c:coff["misc"] + c + 1]

        gcol = sbt(top, "gcol", [128, 2, 8], F32)
        gcolB = Buf("gcol")
        for l in range(2):
            S_.dma("sp", gcol[:, l, :], attn_norm_d[l].rearrange("(k p) -> p k", p=128), writes=[gcolB],
                   allow_slow_non_contiguous=True)
        gffn = sbt(top, "gffn", [128, 2, D], F32)
        gffnB = Buf("gffn")
        for l in range(2):
            S_.dma("sp", gffn[:, l, :], ffn_norm_d[l].partition_broadcast(128), writes=[gffnB])
        gfin = sbt(top, "gfin", [128, D], F32)
        gfinB = Buf("gfin")
        S_.dma("sp", gfin[:], final_norm_d.partition_broadcast(128), writes=[gfinB])
        wr = sbt(top, "wr", [128, 2, 8, 72], F32)
        wrB = Buf("wr")
        for l in range(2):
            S_.dma("sp", wr[:, l, :, 0:8], moe_w_group_d[l].rearrange("(k p) n -> p k n", p=128), writes=[wrB],
                   allow_slow_non_contiguous=True)
            S_.dma("sp", wr[:, l, :, 8:72], moe_w_expert_d[l].rearrange("(k p) n -> p k n", p=128), writes=[wrB],
                   allow_slow_non_contiguous=True)

        E12 = sbt(top, "E12", [128, 2, NT], F32)
        E12B = Buf("E12")
        R12 = sbt(top, "R12", [128, 2, NT], F32)
        G12 = sbt(top, "G12", [128, 2, NT], F32)
        base = sbt(top, "base", [128, 64], F32)
        R12B, G12B, baseB = Buf("R12"), Buf("G12"), Buf("base")
        DEST = sbt(top, "DEST", [128, 2, NT], I32)
        DESTB = Buf("DEST")

        X1B = [Buf("X1_%d" % t) for t in range(NT)]
        X3B = [Buf("X3_%d" % t) for t in range(NT)]
        H2B = [[Buf("H2_%d_%d" % (l, t)) for t in range(NT + 1)] for l in range(2)]
        H2allB = [Buf("H2all%d" % l) for l in range(2)]
        YtB = [[Buf("Y_%d_%d" % (l, b)) for b in range(NB * SPB)] for l in range(2)]
        YallB = [Buf("Yall%d" % l) for l in range(2)]
        RECB = [Buf("REC%d" % l) for l in range(2)]
        outB = [Buf("out%d" % t) for t in range(NT)]

        def rms_stats(stack_tiles, xt, xtB, n_free, tag):
            junk, junkB, ss, ssB, rstd, rstdB = stack_tiles
            A(lambda e: e.activation(junk[:, 0:n_free], xt, AF.Square, accum_out=ss[:]), [xtB], [junkB, ssB])
            A(lambda e: e.activation(rstd[:], ss[:], AF.Ln, bias=EPS, scale=1.0 / n_free), [ssB], [rstdB])
            A(lambda e: e.activation(rstd[:], rstd[:], AF.Exp, scale=-0.5), [rstdB], [rstdB])

        def moe_pre(l, t, x1t, x1tB, W):
            rms_stats((W["junk"], W["junkB"], W["ss"], W["ssB"], W["rstd"], W["rstdB"]), x1t[:], x1tB, D, "mp")
            h2, h2B = W["h2"], W["h2B"]
            V(lambda e: e.scalar_tensor_tensor(h2[:], x1t[:], W["rstd"][:], gffn[:, l, :], ALU.mult, ALU.mult),
              [x1tB, W["rstdB"], gffnB], [h2B])
            h2b, h2bB = W["h2b"], W["h2bB"]
            G(lambda e: e.tensor_copy(h2b[:].rearrange("t (k p) -> t k p", k=8), h2[:].rearrange("t (p k) -> t k p", k=8)),
              [h2B], [h2bB])
            S_.dma("sp", H2_d[l][t * 128:(t + 1) * 128, :], h2b[:], reads=[h2bB], writes=[H2B[l][t]])
            h2T, h2TB = W["h2T"], W["h2TB"]
            for half in range(2):
                pb, pbB = bank()
                for kk in range(4):
                    kc = half * 4 + kk
                    P(lambda e, kc=kc, kk=kk, pb=pb: e.transpose(pb[:, kk * 128:(kk + 1) * 128], h2[:, kc * 128:(kc + 1) * 128], CM("ident")),
                      [h2B, cmB], [pbB])
                A(lambda e, pb=pb, half=half: e.copy(h2T[:, half * 4:(half + 1) * 4, :], pb[:].rearrange("p (k t) -> p k t", k=4)),
                  [pbB], [h2TB])
            pl, plB = bank()
            for kc in range(8):
                P(lambda e, kc=kc: e.matmul(pl[:, 0:72], h2T[:, kc, :], wr[:, l, kc, :], start=(kc == 0), stop=(kc == 7)),
                  [h2TB, wrB], [plB])
            lg, lgB = W["lg"], W["lgB"]
            V(lambda e: e.tensor_copy(lg[:], pl[:, 0:72]), [plB], [lgB])
            sm, smB = W["sm"], W["smB"]
            t8, t8B = W["t8"], W["t8B"]
            c8, c8B = W["c8"], W["c8B"]
            gm, ngm, gsum, gw, m1, m2, dd, ed, den, g1, g2 = [sm[:, i:i + 1] for i in range(11)]
            V(lambda e: e.tensor_reduce(gm, lg[:, 0:8], AX.X, ALU.max), [lgB], [smB])
            V(lambda e: e.tensor_scalar(c8[:, 0, :], lg[:, 0:8], gm, None, ALU.is_equal), [lgB, smB], [c8B])
            V(lambda e: e.tensor_scalar(ngm, gm, -1.0, None, ALU.mult), [smB], [smB])
            A(lambda e: e.activation(c8[:, 3, :], lg[:, 0:8], AF.Exp, bias=ngm, scale=1.0, accum_out=gsum), [lgB, smB], [c8B, smB])
            V(lambda e: e.reciprocal(gw, gsum), [smB], [smB])
            V(lambda e: e.tensor_tensor(t8[:], lg[:, 8:72].rearrange("p (g e) -> p g e", g=8),
                                        c8[:, 0, :].unsqueeze(2).to_broadcast([128, 8, 8]), ALU.mult), [lgB, c8B], [t8B])
            V(lambda e: e.tensor_reduce(c8[:, 1, :], t8[:].rearrange("p g e -> p e g"), AX.X, ALU.add), [t8B], [c8B])
            V(lambda e: e.tensor_reduce(m1, c8[:, 1, :], AX.X, ALU.max), [c8B], [smB])
            V(lambda e: e.tensor_scalar(c8[:, 2, :], c8[:, 1, :], m1, None, ALU.is_equal), [c8B, smB], [c8B])
            V(lambda e: e.scalar_tensor_tensor(c8[:, 1, :], c8[:, 2, :], -1e30, c8[:, 1, :], ALU.mult, ALU.add), [c8B], [c8B])
            V(lambda e: e.tensor_reduce(m2, c8[:, 1, :], AX.X, ALU.max), [c8B], [smB])
            V(lambda e: e.tensor_scalar(c8[:, 3, :], c8[:, 1, :], m2, None, ALU.is_equal), [c8B, smB], [c8B])
            V(lambda e: e.tensor_tensor(dd, m2, m1, ALU.subtract), [smB], [smB])
            A(lambda e: e.activation(ed, dd, AF.Exp), [smB], [smB])
            V(lambda e: e.tensor_scalar(den, ed, 1.0, None, ALU.add), [smB], [smB])
            V(lambda e: e.reciprocal(den, den), [smB], [smB])
            V(lambda e: e.tensor_tensor(G12[:, 0, t:t + 1], gw, den, ALU.mult), [smB], [G12B])
            V(lambda e: e.tensor_tensor(G12[:, 1, t:t + 1], G12[:, 0, t:t + 1], ed, ALU.mult), [smB, G12B], [G12B])
            oh, ohB = W["oh"], W["ohB"]
            for k, ci in ((0, 2), (1, 3)):
                V(lambda e, k=k, ci=ci: e.tensor_tensor(oh[:, k, :].rearrange("p (g e) -> p g e", g=8),
                                                        c8[:, 0, :].unsqueeze(2).to_broadcast([128, 8, 8]),
                                                        c8[:, ci, :].unsqueeze(1).to_broadcast([128, 8, 8]), ALU.mult),
                  [c8B], [ohB])
            Mb, MbB = W["Mb"], W["MbB"]
            V(lambda e: e.tensor_tensor(Mb[:], oh[:, 0, :], oh[:, 1, :], ALU.add), [ohB], [MbB])
            pr, prB = bank()
            P(lambda e: e.matmul(pr[:, 0:64], CMb("SLT"), Mb[:], start=True, stop=True), [MbB, cmbB], [prB])
            P(lambda e: e.matmul(pr[:, 64:128], CMb("ONES"), Mb[:], start=True, stop=True), [MbB, cmbB], [prB])
            rk, rkB = W["rk"], W["rkB"]
            V(lambda e: e.tensor_tensor(rk[:, 0, :], pr[:, 0:64], base[:], ALU.add), [prB, baseB], [rkB])
            V(lambda e: e.tensor_tensor(base[:], pr[:, 64:128], base[:], ALU.add), [prB, baseB], [baseB])
            for k in range(2):
                V(lambda e, k=k: e.tensor_tensor(rk[:, 1, :], rk[:, 0, :], oh[:, k, :], ALU.mult), [rkB, ohB], [rkB])
                V(lambda e, k=k: e.tensor_reduce(R12[:, k, t:t + 1], rk[:, 1, :], AX.X, ALU.add), [rkB], [R12B])
                V(lambda e, k=k: e.tensor_tensor(rk[:, 1, :], cv[:, 16 + NB:16 + NB + 64], oh[:, k, :], ALU.mult), [cvB, ohB], [rkB])
                V(lambda e, k=k: e.tensor_reduce(E12[:, k, t:t + 1], rk[:, 1, :], AX.X, ALU.add), [rkB], [E12B])

        def moe_pre_tiles(stack):
            W = {}

            def mk(name, shape, dt):
                W[name] = sbt(stack, "mp_" + name, shape, dt)
                W[name + "B"] = Buf("mp_" + name)
            mk("junk", [128, D], F32); mk("ss", [128, 1], F32); mk("rstd", [128, 1], F32)
            mk("h2", [128, D], F32); mk("h2b", [128, D], BF16); mk("h2T", [128, 8, 128], F32)
            mk("lg", [128, 72], F32); mk("sm", [128, 16], F32); mk("t8", [128, 8, 8], F32); mk("c8", [128, 4, 8], F32)
            mk("Mb", [128, 64], BF16); mk("rk", [128, 2, 64], F32); mk("oh", [128, 2, 64], F32)
            return W

        def dispatch(l):
            with contextlib.ExitStack() as st:
                cnt = base
                t16 = sbt(st, "dp_t16", [128, 64, 16], F32); t16B = Buf()
                pad = sbt(st, "dp_pad", [128, 2, 64], F32); padB = Buf()
                V(lambda e: e.tensor_tensor(t16[:], cnt[:].unsqueeze(2).to_broadcast([128, 64, 16]),
                                            cv[:, 0:16].unsqueeze(1).to_broadcast([128, 64, 16]), ALU.is_gt), [baseB, cvB], [t16B])
                V(lambda e: e.tensor_reduce(pad[:, 0, :], t16[:], AX.X, ALU.add), [t16B], [padB])
                V(lambda e: e.tensor_scalar(pad[:, 0, :], pad[:, 0, :], float(BLK), None, ALU.mult), [padB], [padB])
                cs = sbt(st, "dp_cs", [128, 2, 64], F32); csB = Buf()
                V(lambda e: e.tensor_copy(cs[:, 0, :], pad[:, 0, :]), [padB], [csB])
                cur = 0
                sh = 1
                while sh < 64:
                    nxt = 1 - cur
                    V(lambda e, cur=cur, nxt=nxt, sh=sh: e.tensor_copy(cs[:, nxt, 0:sh], cs[:, cur, 0:sh]), [csB], [csB])
                    V(lambda e, cur=cur, nxt=nxt, sh=sh: e.tensor_tensor(cs[:, nxt, sh:64], cs[:, cur, sh:64], cs[:, cur, 0:64 - sh], ALU.add), [csB], [csB])
                    cur = nxt
                    sh *= 2
                pend = cs[:, cur, :]
                V(lambda e: e.tensor_tensor(pad[:, 1, :], pend, pad[:, 0, :], ALU.subtract), [csB, padB], [padB])
                big = sbt(st, "dp_big", [128, NT, 64], F32); bigB = Buf()
                dst = sbt(st, "dp_dst", [128, 2, NT], F32); dstB = Buf()
                for k in range(2):
                    V(lambda e, k=k: e.tensor_tensor(big[:], cv[:, 16 + NB:16 + NB + 64].unsqueeze(1).to_broadcast([128, NT, 64]),
                                                     E12[:, k, :].unsqueeze(2).to_broadcast([128, NT, 64]), ALU.is_equal), [cvB, E12B], [bigB])
                    V(lambda e: e.tensor_tensor(big[:], big[:], pad[:, 1, :].unsqueeze(1).to_broadcast([128, NT, 64]), ALU.mult),
                      [bigB, padB], [bigB])
                    V(lambda e, k=k: e.tensor_reduce(dst[:, k, :], big[:], AX.X, ALU.add), [bigB], [dstB])
                V(lambda e: e.tensor_tensor(dst[:], dst[:], R12[:], ALU.add), [dstB, R12B], [dstB])
                V(lambda e: e.tensor_copy(DEST[:], dst[:]), [dstB], [DESTB])
                be3 = sbt(st, "dp_be3", [128, NB, 64], F32); be3B = Buf()
                bef = sbt(st, "dp_bef", [128, NB], F32); befB = Buf()
                V(lambda e: e.tensor_tensor(be3[:], pend.unsqueeze(1).to_broadcast([128, NB, 64]),
                                            cv[:, 16:16 + NB].unsqueeze(2).to_broadcast([128, NB, 64]), ALU.is_le), [csB, cvB], [be3B])
                V(lambda e: e.tensor_reduce(bef[:], be3[:], AX.X, ALU.add), [be3B], [befB])
                V(lambda e: e.tensor_scalar(bef[:], bef[:], 63.0, None, ALU.min), [befB], [befB])
                wf = sbt(st, "dp_wf", [128, 3, NB], F32); wfB = Buf()
                V(lambda e: e.tensor_scalar(wf[:, 0, :], bef[:], 128.0, misc(2), ALU.mult, ALU.add), [befB, cmB], [wfB])
                V(lambda e: e.tensor_scalar(wf[:, 1, :], bef[:], 256.0, misc(2), ALU.mult, ALU.add), [befB, cmB], [wfB])
                V(lambda e: e.tensor_scalar(wf[:, 2, :], bef[:], 256.0, misc(3), ALU.mult, ALU.add), [befB, cmB], [wfB])
                emp = sbt(st, "dp_emp", [128, NB], F32); empB = Buf()
                V(lambda e: e.tensor_scalar(emp[:], cv[:, 16:16 + NB], cs[:, cur, 63:64], 1.0e5, ALU.is_ge, ALU.mult), [cvB, csB], [empB])
                for k in range(3):
                    V(lambda e, k=k: e.tensor_tensor(wf[:, k, :], wf[:, k, :], emp[:], ALU.add), [wfB, empB], [wfB])
                if l:
                    V(lambda e: e.tensor_scalar(wf[:, 0, :], wf[:, 0, :], float(l * 64 * 128), None, ALU.add), [wfB], [wfB])
                    V(lambda e: e.tensor_scalar(wf[:, 1:3, :], wf[:, 1:3, :], float(l * 64 * 256), None, ALU.add), [wfB], [wfB])
                V(lambda e: e.tensor_copy(WIDX[:], wf[:]), [wfB], [WIDXB])
                rec = sbt(st, "dp_rec", [128, 2, NT, 2], I32); recB = Buf()
                fill = sbt(st, "dp_fill", [128, NSLOT // 128, 2], I32); fillB = Buf()
                V(lambda e: e.memset(fill[:], 0), [], [fillB])
                V(lambda e: e.memset(fill[:, :, 0:1], 1 << 20), [fillB], [fillB])
                S_.dma("sp", REC_d[l].rearrange("(p n) c -> p n c", p=128), fill[:], reads=[fillB], writes=[RECB[l]])
                for k in range(2):
                    V(lambda e, k=k: e.tensor_copy(rec[:, k, :, 0], tokidx[:]), [tokidxB], [recB])
                    V(lambda e, k=k: e.tensor_copy(rec[:].bitcast(F32)[:, k, :, 1], G12[:, k, :]), [G12B], [recB])
                for t in range(NT):
                    for k in range(2):
                        S_.dma("pool", REC_d[l][:, :], rec[:, k, t, :], reads=[recB, DESTB, RECB[l]], writes=[],
                               indirect=dict(out_offset=bass.IndirectOffsetOnAxis(ap=DEST[:, k, t:t + 1], axis=0), in_offset=None))
                S_.fence("pool", [RECB[l]], RECB[l])
                S_.fence("pool", H2B[l], H2allB[l])
                S_.barrier()

        REGS = [nc.gpsimd.to_reg(v) for v in (2 * 64 * 128 - 1, 2 * 64 * 256 - 1, S - 1, NSLOT - 1)]
        WIDX = sbt(top, "WIDX", [128, 3, NB], I32)
        WIDXB = Buf("WIDX")

        def experts(l):
            w1v = moe_w1_d.rearrange("l e (p k) n -> (l e p) (k n)", k=8)
            w3v = moe_w3_d.rearrange("l e (p k) n -> (l e p) (k n)", k=8)
            w2v = moe_w2_d.rearrange("l e f n -> (l e f) n")
            with contextlib.ExitStack() as st:
                NBUF = 5
                PFD = 3
                w1 = [sbt(st, "ex_w1_%d" % i, [128, 8, 256], BF16) for i in range(NBUF)]
                w3 = [sbt(st, "ex_w3_%d" % i, [128, 8, 256], BF16) for i in range(NBUF)]
                w2 = [sbt(st, "ex_w2_%d" % i, [128, 2, D], BF16) for i in range(NBUF)]
                wB = [[Buf(), Buf(), Buf(), Buf()] for i in range(NBUF)]
                rec = [sbt(st, "ex_rec_%d" % i, [128, SPB, 2], I32) for i in range(NBUF)]
                recB = [Buf() for i in range(NBUF)]
                xs = [sbt(st, "ex_xs_%d" % i, [128, SPB, D], BF16) for i in range(NBUF)]
                xsB = [[Buf() for j in range(SPB)] for i in range(NBUF)]
                xsT = [sbt(st, "ex_xsT%d" % i, [128, 8, BLK], BF16) for i in range(2)]; xsTB = [Buf(), Buf()]
                sil = [sbt(st, "ex_sil%d" % i, [128, BLK], BF16) for i in range(2)]; silB = [Buf(), Buf()]
                hid = [sbt(st, "ex_hid%d" % i, [128, 2, BLK], BF16) for i in range(2)]; hidB = [Buf(), Buf()]
                yo = [sbt(st, "ex_yo_%d" % i, [128, D], BF16) for i in range(8)]
                yoB = [[Buf(), Buf()] for i in range(8)]

                yidx = [sbt(st, "ex_yidx_%d" % i, [128, SPB], I32) for i in range(NBUF)]
                yidf = [sbt(st, "ex_yidf_%d" % i, [128, SPB], F32) for i in range(NBUF)]
                yidxB = [Buf() for i in range(NBUF)]
                yidfB = [Buf() for i in range(NBUF)]
                for i in range(NBUF):
                    V(lambda e, i=i: e.memset(xs[i][:], 0.0), [], xsB[i])
                YR0 = 16 + NB + 64
                NW1m, NW2m, Sm, NSLOTm = REGS

                def load(b):
                    i = b % NBUF
                    S_.dma("act", rec[i][:], REC_d[l][b * BLK:(b + 1) * BLK, :].rearrange("(j p) c -> p j c", p=128),
                           reads=[RECB[l]], writes=[recB[i]])
                    S_.dma("pool", w1[i][:].rearrange("p k n -> p (k n)"), w1v, reads=[WIDXB], writes=[wB[i][0]],
                           indirect=dict(out_offset=None, in_offset=bass.IndirectOffsetOnAxis(ap=WIDX[:, 0, b:b + 1], axis=0),
                                         bounds_check=NW1m, oob_is_err=False))
                    S_.dma("pool", w3[i][:].rearrange("p k n -> p (k n)"), w3v, reads=[WIDXB], writes=[wB[i][1]],
                           indirect=dict(out_offset=None, in_offset=bass.IndirectOffsetOnAxis(ap=WIDX[:, 0, b:b + 1], axis=0),
                                         bounds_check=NW1m, oob_is_err=False))
                    for fc in range(2):
                        S_.dma("pool", w2[i][:, fc, :], w2v, reads=[WIDXB], writes=[wB[i][2 + fc]],
                               indirect=dict(out_offset=None, in_offset=bass.IndirectOffsetOnAxis(ap=WIDX[:, 1 + fc, b:b + 1], axis=0),
                                             bounds_check=NW2m, oob_is_err=False))
                    for j in range(SPB):
                        S_.dma("pool", xs[i][:, j, :], H2_d[l][:, :], reads=[recB[i], H2allB[l]], writes=[xsB[i][j]],
                               indirect=dict(out_offset=None, in_offset=bass.IndirectOffsetOnAxis(ap=rec[i][:, j, 0:1], axis=0),
                                             bounds_check=Sm, oob_is_err=False))

                def stT(b):
                    i = b % NBUF
                    x2i = b % 2
                    for j in range(SPB):
                        pb, pbB = bank()
                        pbb = pb[:].bitcast(BF16)
                        for kc in range(8):
                            P(lambda e, kc=kc, j=j, pbb=pbb: e.transpose(pbb[:, kc * 128:(kc + 1) * 128], xs[i][:, j, kc * 128:(kc + 1) * 128], CMb("ident")),
                              [xsB[i][j], cmbB], [pbB])
                        if j % 2 == 0:
                            V(lambda e, j=j, pbb=pbb: e.tensor_copy(xsT[x2i][:, :, j * 128:(j + 1) * 128], pbb.rearrange("p (k t) -> p k t", k=8)),
                              [pbB], [xsTB[x2i]])
                        else:
                            A(lambda e, j=j, pbb=pbb: e.copy(xsT[x2i][:, :, j * 128:(j + 1) * 128], pbb.rearrange("p (k t) -> p k t", k=8)),
                              [pbB], [xsTB[x2i]])

                def stH(b):
                    i = b % NBUF
                    x2i = b % 2
                    for m in range(2):
                        p1, p1B = bank()
                        for kc in range(8):
                            P(lambda e, kc=kc, m=m, p1=p1: e.matmul(p1[:, 0:BLK], w1[i][:, kc, m * 128:(m + 1) * 128], xsT[x2i][:, kc, :], start=(kc == 0), stop=(kc == 7)),
                              [wB[i][0], xsTB[x2i]], [p1B])
                        A(lambda e, m=m, p1=p1: e.activation(sil[m][:], p1[:, 0:BLK], AF.Silu), [p1B], [silB[m]])
                        p3, p3B = bank()
                        for kc in range(8):
                            P(lambda e, kc=kc, m=m, p3=p3: e.matmul(p3[:, 0:BLK], w3[i][:, kc, m * 128:(m + 1) * 128], xsT[x2i][:, kc, :], start=(kc == 0), stop=(kc == 7)),
                              [wB[i][1], xsTB[x2i]], [p3B])
                        V(lambda e, m=m, p3=p3: e.tensor_tensor(hid[x2i][:, m, :], p3[:, 0:BLK], sil[m][:], ALU.mult), [p3B, silB[m]], [hidB[x2i]])

                def stY(b):
                    i = b % NBUF
                    x2i = b % 2
                    for j in range(SPB):
                        yi = (b * SPB + j) % len(yo)
                        for half in range(2):
                            py, pyB = bank()
                            for fc in range(2):
                                P(lambda e, fc=fc, j=j, half=half, py=py: e.matmul(py[:], hid[x2i][:, fc, j * 128:(j + 1) * 128], w2[i][:, fc, half * 512:(half + 1) * 512],
                                                                              start=(fc == 0), stop=(fc == 1)), [hidB[x2i], wB[i][2 + fc]], [pyB])
                            gate = rec[i][:].bitcast(F32)[:, j, 1:2]
                            if half == 0:
                                V(lambda e, py=py, yi=yi, gate=gate: e.tensor_scalar(yo[yi][:, 0:512], py[:], gate, None, ALU.mult), [pyB, recB[i]], [yoB[yi][0]])
                            else:
                                A(lambda e, py=py, yi=yi, gate=gate: e.activation(yo[yi][:, 512:1024], py[:], AF.Copy, scale=gate), [pyB, recB[i]], [yoB[yi][1]])
                        row = (b * SPB + j) * 128
                        S_.dma("sp", Y_d[l][row:row + 128, :], yo[yi][:], reads=[yoB[yi][0], yoB[yi][1]], writes=[YtB[l][b * SPB + j]])

                for b0 in range(min(PFD, NB)):
                    load(b0)
                stT(0)
                for b in range(NB + 1):
                    if b + 1 < NB:
                        stT(b + 1)
                    if b < NB:
                        stH(b)
                    if b - 1 >= 0:
                        stY(b - 1)
                    if b + PFD < NB:
                        load(b + PFD)
                S_.fence("pool", YtB[l], YallB[l])
                S_.barrier()

        def combine(l, t, xold, xoldB, ya, yaB, xo, xoB):
            for k in range(2):
                S_.dma("pool", ya[k][:], Y_d[l][:, :], reads=[DESTB, YallB[l]], writes=[yaB[k]],
                       indirect=dict(out_offset=None, in_offset=bass.IndirectOffsetOnAxis(ap=DEST[:, k, t:t + 1], axis=0)))
            V(lambda e: e.tensor_tensor(xo[:], xold[:], ya[0][:], ALU.add), [xoldB, yaB[0]], [xoB])
            G(lambda e: e.tensor_tensor(xo[:], xo[:], ya[1][:], ALU.add), [xoB, yaB[1]], [xoB])

        def phase_gla():
            with contextlib.ExitStack() as st:
                win = sbt(st, "g_win", [128, 8, 3088], BF16); winB = Buf()
                for kc in range(8):
                    S_.dma("pool", win[:, kc, :], gla_w_in_d[0, kc * 128:(kc + 1) * 128, :], writes=[winB])
                wo = sbt(st, "g_wo", [128, 8, D], BF16); woB = Buf()
                for kc in range(8):
                    S_.dma("pool", wo[:, kc, :], gla_w_o_d[0, kc * 128:(kc + 1) * 128, :], writes=[woB])
                wg = sbt(st, "g_wg", [32, 512], F32); wgB = Buf()
                V(lambda e: e.memset(wg[:], 0.0), [], [wgB])
                S_.dma("sp", wg[0:16, :], gla_w_gate_d[0], writes=[wgB])
                S_.dma("sp", wg[16:17, :], gla_b_gate_d[0:1, :], writes=[wgB])
                onrm = sbt(st, "g_onrm", [128, 256], F32); onrmB = Buf()
                S_.dma("sp", onrm[:], gla_out_norm_d[0].partition_broadcast(128), writes=[onrmB])
                alrT = sbt(st, "g_alrT", [32, 512], F32); alrTB = Buf()
                V(lambda e: e.memset(alrT[:], 1.0), [], [alrTB])
                Sf = [[sbt(st, "g_Sf%d_%d" % (h, i), [128, 256], F32) for i in range(2)] for h in range(4)]
                SfB = [[Buf(), Buf()] for h in range(4)]
                Sb = [[sbt(st, "g_Sb%d_%d" % (h, i), [128, 256], BF16) for i in range(3)] for h in range(4)]
                SbB = [[Buf(), Buf(), Buf()] for h in range(4)]
                for h in range(4):
                    V(lambda e, h=h: e.memset(Sf[h][0][:], 0.0), [], [SfB[h][0]])
                    V(lambda e, h=h: e.memset(Sb[h][0][:], 0.0), [], [SbB[h][0]])
                sf_i = [0] * 4
                sb_i = [0] * 4
                xt = [sbt(st, "g_xt%d" % j, [128, D], F32) for j in range(4)]
                xtB = [Buf() for j in range(4)]
                junk = sbt(st, "g_junk", [128, D], F32); junkB = Buf()
                ss = sbt(st, "g_ss", [128, 1], F32); ssB = Buf()
                rstd = sbt(st, "g_rstd", [128, 1], F32); rstdB = Buf()
                xs = sbt(st, "g_xs", [128, D], BF16); xsB = Buf()
                hT = sbt(st, "g_hT", [128, 8, 512], BF16); hTB = Buf()
                qT = sbt(st, "g_qT", [128, 4, 512], BF16); qTB = Buf()
                kT = sbt(st, "g_kT", [128, 4, 512], BF16); kTB = Buf()
                e1 = sbt(st, "g_e1", [128, 512], F32); e1B = Buf()
                la = sbt(st, "g_la", [128, 512], F32); laB = Buf()
                E = [sbt(st, "g_E%d" % i, [128, 512], F32) for i in range(4)]
                EB = [Buf() for i in range(4)]
                Ed = sbt(st, "g_Ed", [128, 8], F32); EdB = Buf()
                q1T = sbt(st, "g_q1T", [128, 4, 128], BF16); q1TB = Buf()
                k1T = sbt(st, "g_k1T", [128, 4, 128], BF16); k1TB = Buf()
                q2T = sbt(st, "g_q2T", [128, 4, 128], BF16); q2TB = Buf()
                k2 = sbt(st, "g_k2", [128, 512], BF16); k2B = Buf()
                vt = sbt(st, "g_v", [128, D], BF16); vB = Buf()
                gr = sbt(st, "g_gr", [128, D], F32); grB = Buf()
                AmT = sbt(st, "g_AmT", [128, 4, 128], BF16); AmTB = Buf()
                ssq = sbt(st, "g_ssq", [128, 4], F32); ssqB = Buf()
                og = sbt(st, "g_og", [128, D], BF16); ogB = Buf()
                ogT = sbt(st, "g_ogT", [128, 8, 128], BF16); ogTB = Buf()
                x1t = sbt(st, "g_x1t", [128, D], F32); x1tB = Buf()
                W = moe_pre_tiles(st)
                V(lambda e: e.memset(base[:], 0.0), [], [baseB])
                SCALE = 128.0 ** -0.5

                for g in range(NG):
                    for j in range(4):
                        t = g * 4 + j
                        S_.dma("sp", xt[j][:], x_d[t * 128:(t + 1) * 128, :], writes=[xtB[j]])
                    for j in range(4):
                        rms_stats((junk, junkB, ss, ssB, rstd, rstdB), xt[j][:], xtB[j], D, "g")
                        V(lambda e, j=j: e.tensor_scalar(xs[:], xt[j][:], rstd[:], None, ALU.mult), [xtB[j], rstdB], [xsB])
                        pb, pbB = bank()
                        pbb = pb[:].bitcast(BF16)
                        for kc in range(8):
                            P(lambda e, kc=kc, pbb=pbb: e.transpose(pbb[:, kc * 128:(kc + 1) * 128], xs[:, kc * 128:(kc + 1) * 128], CMb("ident")),
                              [xsB, cmbB], [pbB])
                        V(lambda e, j=j, pbb=pbb: e.tensor_tensor(hT[:, :, j * 128:(j + 1) * 128], pbb.rearrange("p (k t) -> p k t", k=8),
                                                                  gcol[:, 0, :].unsqueeze(2).to_broadcast([128, 8, 128]), ALU.mult),
                          [pbB, gcolB], [hTB])
                    for m in range(9):
                        pb, pbB = bank()
                        if m < 8:
                            c0, M = m * 128, 128
                        else:
                            c0, M = 2048, 16
                        for kc in range(8):
                            P(lambda e, kc=kc, c0=c0, M=M, pb=pb: e.matmul(pb[0:M, :], win[:, kc, c0:c0 + M], hT[:, kc, :], start=(kc == 0), stop=(kc == 7)),
                              [winB, hTB], [pbB])
                        if m < 4:
                            A(lambda e, m=m, pb=pb: e.copy(qT[:, m, :], pb[:]), [pbB], [qTB])
                        elif m < 8:
                            V(lambda e, m=m, pb=pb: e.tensor_copy(kT[:, m - 4, :], pb[:]), [pbB], [kTB])
                        else:
                            V(lambda e, pb=pb: e.tensor_copy(alrT[0:16, :], pb[0:16, :]), [pbB], [alrTB])
                    def tile_head(j):
                        t = g * 4 + j
                        js = slice(j * 128, (j + 1) * 128)
                        pz, pzB = bank()
                        P(lambda e, pz=pz, js=js: e.matmul(pz[:], alrT[:, js], wg[:], start=True, stop=True), [alrTB, wgB], [pzB])
                        A(lambda e, pz=pz: e.activation(e1[:], pz[:], AF.Exp, scale=-1.0), [pzB], [e1B])
                        A(lambda e: e.activation(la[:], e1[:], AF.Ln, bias=1.0), [e1B], [laB])
                        pq, pqB = bank()
                        pg, pgB = bank()
                        pk, pkB = bank()
                        pd, pdB = bank()
                        for h in range(4):
                            hs = slice(h * 128, (h + 1) * 128)
                            P(lambda e, hs=hs, pq=pq: e.matmul(pq[:, hs], la[:, hs], CM("TQ"), start=True, stop=True), [laB, cmB], [pqB])
                            P(lambda e, hs=hs, pg=pg: e.matmul(pg[:, hs], la[:, hs], CM("TG"), start=True, stop=True), [laB, cmB], [pgB])
                            P(lambda e, hs=hs, h=h, pd=pd: e.matmul(pd[:, 2 * h:2 * h + 2], la[:, hs], CM("CSG", 2), start=True, stop=True), [laB, cmB], [pdB])
                        P(lambda e, pk=pk: e.matmul(pk[:], CM("TAFT"), la[:], start=True, stop=True), [laB, cmB], [pkB])
                        A(lambda e, pq=pq: e.activation(E[0][:], pq[:], AF.Exp), [pqB], [EB[0]])
                        A(lambda e, pq=pq: e.activation(E[1][:], pq[:], AF.Exp, scale=-1.0), [pqB], [EB[1]])
                        A(lambda e, pg=pg: e.activation(E[2][:], pg[:], AF.Exp), [pgB], [EB[2]])
                        A(lambda e, pk=pk: e.activation(E[3][:], pk[:], AF.Exp), [pkB], [EB[3]])
                        A(lambda e, pd=pd: e.activation(Ed[:], pd[:, 0:8], AF.Exp), [pdB], [EdB])
                        V(lambda e, js=js: e.scalar_tensor_tensor(q1T[:], qT[:, :, js], SCALE, E[0][:].rearrange("p (h t) -> p h t", h=4), ALU.mult, ALU.mult),
                          [qTB, EB[0]], [q1TB])
                        V(lambda e, js=js: e.tensor_tensor(k1T[:], kT[:, :, js], E[1][:].rearrange("p (h t) -> p h t", h=4), ALU.mult), [kTB, EB[1]], [k1TB])
                        V(lambda e, js=js: e.scalar_tensor_tensor(q2T[:], qT[:, :, js], SCALE, E[2][:].rearrange("p (h t) -> p h t", h=4), ALU.mult, ALU.mult),
                          [qTB, EB[2]], [q2TB])
                        pkk, pkkB = bank()
                        for kc in range(8):
                            P(lambda e, kc=kc, pkk=pkk, js=js: e.matmul(pkk[:], hT[:, kc, js], win[:, kc, 512:1024], start=(kc == 0), stop=(kc == 7)), [hTB, winB], [pkkB])
                        V(lambda e, pkk=pkk: e.tensor_tensor(k2[:], pkk[:], E[3][:], ALU.mult), [pkkB, EB[3]], [k2B])
                        for half in range(2):
                            pv, pvB = bank()
                            for kc in range(8):
                                P(lambda e, kc=kc, pv=pv, js=js, half=half: e.matmul(pv[:], hT[:, kc, js], win[:, kc, 1024 + half * 512:1536 + half * 512],
                                                                                 start=(kc == 0), stop=(kc == 7)), [hTB, winB], [pvB])
                            A(lambda e, pv=pv, half=half: e.copy(vt[:, half * 512:(half + 1) * 512], pv[:]), [pvB], [vB])
                        for half in range(2):
                            pr_, prB_ = bank()
                            for kc in range(8):
                                P(lambda e, kc=kc, pr_=pr_, js=js, half=half: e.matmul(pr_[:], hT[:, kc, js], win[:, kc, 2064 + half * 512:2576 + half * 512],
                                                                                   start=(kc == 0), stop=(kc == 7)), [hTB, winB], [prB_])
                            A(lambda e, pr_=pr_, half=half: e.activation(gr[:, half * 512:(half + 1) * 512], pr_[:], AF.Silu), [prB_], [grB])
                        G(lambda e: e.tensor_tensor(gr[:].rearrange("p (h v) -> p h v", h=4), gr[:].rearrange("p (h v) -> p h v", h=4),
                                                    onrm[:].unsqueeze(1).to_broadcast([128, 4, 256]), ALU.mult), [grB, onrmB], [grB])
                        pa, paB = bank()
                        for h in range(4):
                            P(lambda e, h=h, pa=pa: e.matmul(pa[:, h * 128:(h + 1) * 128], k1T[:, h, :], q1T[:, h, :], start=True, stop=True), [k1TB, q1TB], [paB])
                        V(lambda e, pa=pa: e.tensor_tensor(AmT[:], pa[:].rearrange("p (h t) -> p h t", h=4),
                                                           CM("MASK").unsqueeze(1).to_broadcast([128, 4, 128]), ALU.mult), [paB, cmB], [AmTB])

                    def tile_mid(j):
                        t = g * 4 + j
                        js = slice(j * 128, (j + 1) * 128)
                        for hp in range(2):
                            po, poB = bank()
                            for hh in range(2):
                                h = hp * 2 + hh
                                hs = slice(h * 128, (h + 1) * 128)
                                vs = slice(h * 256, (h + 1) * 256)
                                osl = slice(hh * 256, (hh + 1) * 256)
                                sb_prev = sb_i[h]
                                P(lambda e, h=h, vs=vs, osl=osl, po=po: e.matmul(po[0:64, osl], AmT[:, h, 0:64], vt[:, vs], start=True, stop=False), [AmTB, vB], [poB])
                                P(lambda e, h=h, osl=osl, po=po, sb_prev=sb_prev: e.matmul(po[0:64, osl], q2T[:, h, 0:64], Sb[h][sb_prev][:], start=False, stop=True),
                                  [q2TB, SbB[h][sb_prev]], [poB])
                                for c in range(2):
                                    pkv, pkvB = bank()
                                    cs_ = slice(c * 64, (c + 1) * 64)
                                    P(lambda e, cs_=cs_, hs=hs, vs=vs, pkv=pkv: e.matmul(pkv[:, 0:256], k2[cs_, hs], vt[cs_, vs], start=True, stop=True), [k2B, vB], [pkvB])
                                    so, sn = sf_i[h], 1 - sf_i[h]
                                    V(lambda e, h=h, c=c, so=so, sn=sn, pkv=pkv: e.scalar_tensor_tensor(Sf[h][sn][:], Sf[h][so][:], Ed[:, 2 * h + c:2 * h + c + 1], pkv[:, 0:256], ALU.mult, ALU.add),
                                      [SfB[h][so], EdB, pkvB], [SfB[h][sn]])
                                    sf_i[h] = sn
                                    sbn = (sb_i[h] + 1) % 3
                                    G(lambda e, h=h, sn=sn, sbn=sbn: e.tensor_copy(Sb[h][sbn][:], Sf[h][sn][:]), [SfB[h][sn]], [SbB[h][sbn]])
                                    sb_i[h] = sbn
                                    if c == 0:
                                        P(lambda e, h=h, vs=vs, osl=osl, po=po: e.matmul(po[64:128, osl], AmT[:, h, 64:128], vt[:, vs], start=True, stop=False), [AmTB, vB], [poB])
                                        P(lambda e, h=h, osl=osl, po=po, sbn=sbn: e.matmul(po[64:128, osl], q2T[:, h, 64:128], Sb[h][sbn][:], start=False, stop=True),
                                          [q2TB, SbB[h][sbn]], [poB])
                                A(lambda e, h=h, osl=osl, po=po: e.activation(junk[:, 0:256], po[:, osl], AF.Square, accum_out=ssq[:, h:h + 1]), [poB], [junkB, ssqB])
                            A(lambda e, hp=hp: e.activation(ssq[:, 2 * hp:2 * hp + 2], ssq[:, 2 * hp:2 * hp + 2], AF.Ln, bias=EPS, scale=1.0 / 256), [ssqB], [ssqB])
                            A(lambda e, hp=hp: e.activation(ssq[:, 2 * hp:2 * hp + 2], ssq[:, 2 * hp:2 * hp + 2], AF.Exp, scale=-0.5), [ssqB], [ssqB])
                            for hh in range(2):
                                h = hp * 2 + hh
                                vs = slice(h * 256, (h + 1) * 256)
                                osl = slice(hh * 256, (hh + 1) * 256)
                                V(lambda e, h=h, vs=vs, osl=osl, po=po: e.scalar_tensor_tensor(og[:, vs], po[:, osl], ssq[:, h:h + 1], gr[:, vs], ALU.mult, ALU.mult),
                                  [poB, ssqB, grB], [ogB])

                    def tile_tail(j):
                        t = g * 4 + j
                        js = slice(j * 128, (j + 1) * 128)
                        pb, pbB = bank()
                        pbb = pb[:].bitcast(BF16)
                        for kc in range(8):
                            P(lambda e, kc=kc, pbb=pbb: e.transpose(pbb[:, kc * 128:(kc + 1) * 128], og[:, kc * 128:(kc + 1) * 128], CMb("ident")), [ogB, cmbB], [pbB])
                        A(lambda e, pbb=pbb: e.copy(ogT[:], pbb.rearrange("p (k t) -> p k t", k=8)), [pbB], [ogTB])
                        for half in range(2):
                            px, pxB = bank()
                            for kc in range(8):
                                P(lambda e, kc=kc, px=px, half=half: e.matmul(px[:], ogT[:, kc, :], wo[:, kc, half * 512:(half + 1) * 512], start=(kc == 0), stop=(kc == 7)),
                                  [ogTB, woB], [pxB])
                            V(lambda e, px=px, half=half, j=j: e.tensor_tensor(x1t[:, half * 512:(half + 1) * 512], px[:], xt[j][:, half * 512:(half + 1) * 512], ALU.add),
                              [pxB, xtB[j]], [x1tB])
                        S_.dma("sp", X1_d[t * 128:(t + 1) * 128, :], x1t[:], reads=[x1tB], writes=[X1B[t]])
                        moe_pre(0, t, x1t, x1tB, W)

                    tile_head(0)
                    tile_mid(0)
                    for j in range(1, 4):
                        tile_head(j)
                        tile_tail(j - 1)
                        tile_mid(j)
                    tile_tail(3)
                zt = sbt(st, "g_zt", [128, D], BF16); ztB = Buf()
                V(lambda e: e.memset(zt[:], 0.0), [], [ztB])
                for l in range(2):
                    S_.dma("sp", H2_d[l][S:S + 128, :], zt[:], reads=[ztB], writes=[H2B[l][NT]])
                S_.barrier()

        def phase_mla():
            with contextlib.ExitStack() as st:
                win = sbt(st, "m_win", [128, 8, 448 + 64], BF16); winB = Buf()
                for kc in range(8):
                    rows = slice(kc * 128, (kc + 1) * 128)
                    S_.dma("pool", win[:, kc, 0:448], mla_w_in_d[0, rows, :], writes=[winB])
                    S_.dma("pool", win[:, kc, 448:480], mla_w_in_d[0, rows, 416:448], writes=[winB])
                    S_.dma("pool", win[:, kc, 480:512], mla_w_in_d[0, rows, 384:416], writes=[winB])
                wuq = sbt(st, "m_wuq", [128, 2, 8, 256], BF16); wuqB = Buf()
                for qc in range(2):
                    rows = slice(qc * 128, (qc + 1) * 128)
                    S_.dma("pool", wuq[:, qc, :, 0:192], mla_w_uq_d[0, rows, :, :], writes=[wuqB])
                    S_.dma("pool", wuq[:, qc, :, 192:224], mla_w_uq_d[0, rows, :, 160:192], writes=[wuqB])
                    S_.dma("pool", wuq[:, qc, :, 224:256], mla_w_uq_d[0, rows, :, 128:160], writes=[wuqB])
                junk = sbt(st, "m_junk", [128, D], F32); junkB = Buf()
                wuk = junk[:].rearrange("p (h n) -> p h n", h=8); wukB = junkB
                S_.dma("sp", wuk, mla_w_uk_d[0], writes=[wukB])
                wukT = sbt(st, "m_wukT", [128, 8, 128], BF16); wukTB = Buf()
                for hq in range(2):
                    pb, pbB = bank()
                    for hh in range(4):
                        h = hq * 4 + hh
                        P(lambda e, h=h, hh=hh, pb=pb: e.transpose(pb[:, hh * 128:(hh + 1) * 128], wuk[:, h, :], CM("ident")), [wukB, cmB], [pbB])
                    V(lambda e, hq=hq, pb=pb: e.tensor_copy(wukT[:, hq * 4:(hq + 1) * 4, :], pb[:].rearrange("p (h c) -> p h c", h=4)), [pbB], [wukTB])
                wuv = sbt(st, "m_wuv", [128, 8, 128], BF16); wuvB = Buf()
                S_.dma("pool", wuv[:], mla_w_uv_d[0], writes=[wuvB])
                wo = sbt(st, "m_wo", [128, 8, D], BF16); woB = Buf()
                for kc in range(8):
                    S_.dma("pool", wo[:, kc, :], mla_w_o_d[0, kc * 128:(kc + 1) * 128, :], writes=[woB])
                qn = sbt(st, "m_qn", [128, 256], F32); qnB = Buf()
                S_.dma("sp", qn[:], mla_q_norm_d[0].partition_broadcast(128), writes=[qnB])
                kvn = sbt(st, "m_kvn", [128, 128], F32); kvnB = Buf()
                S_.dma("sp", kvn[:], mla_kv_norm_d[0].partition_broadcast(128), writes=[kvnB])
                ckvT = sbt(st, "m_ckvT", [128, S], BF16)
                krT = sbt(st, "m_krT", [64, S], BF16)
                Vaug = sbt(st, "m_Vaug", [128, NT, 132], BF16)
                ckvTB = [Buf() for g in range(NG)]
                krTB = [Buf() for g in range(NG)]
                VaugB = [Buf() for g in range(NG)]
                VaB = Buf()
                V(lambda e: e.memset(Vaug[:], 1.0), [], [VaB])
                for g in range(NG):
                    VaugB[g].w = VaB.w
                posi = sbt(st, "m_posi", [64, 512], I32); posiB = Buf()
                cosT = sbt(st, "m_cos", [64, 512], F32); cosB = Buf()
                sinT = sbt(st, "m_sin", [64, 512], F32); sinB = Buf()
                x2 = [sbt(st, "m_x2_%d" % j, [128, D], F32) for j in range(4)]
                x2B = [Buf() for j in range(4)]
                ya = [sbt(st, "m_ya%d" % k, [128, D], F32) for k in range(2)]
                yaB = [Buf(), Buf()]
                ss = sbt(st, "m_ss", [128, 1], F32); ssB = Buf()
                rstd = sbt(st, "m_rstd", [128, 1], F32); rstdB = Buf()
                xs = sbt(st, "m_xs", [128, D], BF16); xsB = Buf()
                hT = sbt(st, "m_hT", [128, 8, 512], BF16); hTB = Buf()
                cq = sbt(st, "m_cq", [128, 256], BF16); cqB = Buf()
                cqT = sbt(st, "m_cqT", [128, 2, 512], BF16); cqTB = Buf()
                ckv = sbt(st, "m_ckv", [128, 128], BF16); ckvB = Buf()
                tmpf = sbt(st, "m_tmpf", [128, 512], F32); tmpfB = Buf()
                tmp2 = sbt(st, "m_tmp2", [128, 512], F32); tmp2B = Buf()
                ang, angB, ang2, ang2B = tmpf[0:64, :], tmpfB, tmp2[0:64, :], tmp2B
                qnT = sbt(st, "m_qnT", [128, 2, 512], BF16); qnTB = [Buf(), Buf()]
                qlatT = sbt(st, "m_qlatT", [128, 2, 512], BF16); qlatTB = [Buf(), Buf()]
                qrT = sbt(st, "m_qrT", [64, 2, 512], BF16); qrTB = [Buf(), Buf()]
                PT = [sbt(st, "m_PT%d" % i, [128, 512], BF16) for i in range(5)]
                PTB = [Buf() for i in range(5)]
                rec_ = sbt(st, "m_recip", [128, 4], F32); rec_B = Buf()
                olat = sbt(st, "m_olat", [128, 4, 128], BF16); olatB = Buf()
                olT = sbt(st, "m_olT", [128, 512], BF16); olTB = Buf()
                oT = sbt(st, "m_oT", [128, 8, 512], BF16); oTB = Buf()
                x3t = sbt(st, "m_x3t", [128, D], F32); x3tB = Buf()
                W = moe_pre_tiles(st)
                print("MLA phase sbuf bytes remaining", nc.sbuf_bytes_remaining, flush=True)
                V(lambda e: e.memset(base[:], 0.0), [], [baseB])
                MSCALE = 192.0 ** -0.5
                TWO_PI = 2.0 * math.pi
                pt_i = [0]

                for g in range(NG):
                    gs = slice(g * 512, (g + 1) * 512)
                    S_.dma("sp", posi[:], pos_d[g * 512:(g + 1) * 512].partition_broadcast(64), writes=[posiB])
                    V(lambda e: e.tensor_copy(ang[:], posi[:]), [posiB], [angB])
                    V(lambda e: e.tensor_scalar(ang[:], ang[:], cm[0:64, coff["misc"]:coff["misc"] + 1], None, ALU.mult), [angB, cmB], [angB])
                    V(lambda e: e.tensor_scalar(ang2[:], ang[:], math.pi / 2, None, ALU.add), [angB], [ang2B])
                    MAGIC = 12582912.0
                    PIS = 3.1415925
                    for (dst, dstB, src, srcB) in ((sinT, sinB, ang, angB), (cosT, cosB, ang2, ang2B)):
                        V(lambda e, dst=dst, src=src: e.tensor_scalar(dst[:], src[:], 1.0 / TWO_PI, MAGIC, ALU.mult, ALU.add), [srcB], [dstB])
                        V(lambda e, dst=dst: e.tensor_scalar(dst[:], dst[:], -MAGIC, None, ALU.add), [dstB], [dstB])
                        V(lambda e, dst=dst, src=src: e.scalar_tensor_tensor(dst[:], dst[:], -TWO_PI, src[:], ALU.mult, ALU.add), [dstB, srcB], [dstB])
                        V(lambda e, dst=dst: e.tensor_scalar(dst[:], dst[:], -PIS, PIS, ALU.max, ALU.min), [dstB], [dstB])
                        A(lambda e, dst=dst: e.activation(dst[:], dst[:], AF.Sin), [dstB], [dstB])
                    V(lambda e: e.tensor_scalar(sinT[:], sinT[:], cm[0:64, coff["misc"] + 1:coff["misc"] + 2], None, ALU.mult), [sinB, cmB], [sinB])
                    for j in range(4):
                        t = g * 4 + j
                        S_.dma("sp", x2[j][:], X1_d[t * 128:(t + 1) * 128, :], reads=[X1B[t]], writes=[x2B[j]])
                    for j in range(4):
                        t = g * 4 + j
                        js = slice(j * 128, (j + 1) * 128)
                        combine(0, t, x2[j], x2B[j], ya, yaB, x2[j], x2B[j])
                        rms_stats((junk, junkB, ss, ssB, rstd, rstdB), x2[j][:], x2B[j], D, "m")
                        V(lambda e, j=j: e.tensor_scalar(xs[:], x2[j][:], rstd[:], None, ALU.mult), [x2B[j], rstdB], [xsB])
                        pb, pbB = bank()
                        pbb = pb[:].bitcast(BF16)
                        for kc in range(8):
                            P(lambda e, kc=kc, pbb=pbb: e.transpose(pbb[:, kc * 128:(kc + 1) * 128], xs[:, kc * 128:(kc + 1) * 128], CMb("ident")), [xsB, cmbB], [pbB])
                        V(lambda e, js=js, pbb=pbb: e.tensor_tensor(hT[:, :, js], pbb.rearrange("p (k t) -> p k t", k=8),
                                                                   gcol[:, 1, :].unsqueeze(2).to_broadcast([128, 8, 128]), ALU.mult), [pbB, gcolB], [hTB])
                        pp, ppB = bank()
                        for kc in range(8):
                            P(lambda e, kc=kc, pp=pp, js=js: e.matmul(pp[:, 0:384], hT[:, kc, js], win[:, kc, 0:384], start=(kc == 0), stop=(kc == 7)), [hTB, winB], [ppB])
                        A(lambda e, pp=pp: e.activation(junk[:, 0:256], pp[:, 0:256], AF.Square, accum_out=ss[:]), [ppB], [junkB, ssB])
                        A(lambda e: e.activation(rstd[:], ss[:], AF.Ln, bias=EPS, scale=1.0 / 256), [ssB], [rstdB])
                        A(lambda e: e.activation(rstd[:], rstd[:], AF.Exp, scale=-0.5), [rstdB], [rstdB])
                        V(lambda e, pp=pp: e.scalar_tensor_tensor(cq[:], pp[:, 0:256], rstd[:], qn[:], ALU.mult, ALU.mult), [ppB, rstdB, qnB], [cqB])
                        A(lambda e, pp=pp: e.activation(junk[:, 0:128], pp[:, 256:384], AF.Square, accum_out=ss[:]), [ppB], [junkB, ssB])
                        A(lambda e: e.activation(rstd[:], ss[:], AF.Ln, bias=EPS, scale=1.0 / 128), [ssB], [rstdB])
                        A(lambda e: e.activation(rstd[:], rstd[:], AF.Exp, scale=-0.5), [rstdB], [rstdB])
                        V(lambda e, pp=pp, t=t: e.scalar_tensor_tensor(Vaug[:, t, 0:128], pp[:, 256:384], rstd[:], kvn[:], ALU.mult, ALU.mult), [ppB, rstdB, kvnB], [VaugB[g]])
                        pb, pbB = bank()
                        pbb = pb[:].bitcast(BF16)
                        for qc in range(2):
                            P(lambda e, qc=qc, pbb=pbb: e.transpose(pbb[:, qc * 128:(qc + 1) * 128], cq[:, qc * 128:(qc + 1) * 128], CMb("ident")), [cqB, cmbB], [pbB])
                        P(lambda e, pbb=pbb, t=t: e.transpose(pbb[:, 256:384], Vaug[:, t, 0:128], CMb("ident")), [VaugB[g], cmbB], [pbB])
                        V(lambda e, js=js, pbb=pbb: e.tensor_copy(cqT[:, :, js], pbb[:, 0:256].rearrange("p (q t) -> p q t", q=2)), [pbB], [cqTB])
                        V(lambda e, t=t, pbb=pbb: e.tensor_copy(ckvT[:, t * 128:(t + 1) * 128], pbb[:, 256:384]), [pbB], [ckvTB[g]])
                    pk1, pk1B = bank()
                    pk2, pk2B = bank()
                    for kc in range(8):
                        P(lambda e, kc=kc, pk1=pk1: e.matmul(pk1[0:64, :], win[:, kc, 384:448], hT[:, kc, :], start=(kc == 0), stop=(kc == 7)), [winB, hTB], [pk1B])
                    for kc in range(8):
                        P(lambda e, kc=kc, pk2=pk2: e.matmul(pk2[0:64, :], win[:, kc, 448:512], hT[:, kc, :], start=(kc == 0), stop=(kc == 7)), [winB, hTB], [pk2B])
                    V(lambda e, pk2=pk2: e.tensor_tensor(tmpf[0:64, :], pk2[0:64, :], sinT[:], ALU.mult), [pk2B, sinB], [tmpfB])
                    V(lambda e, pk1=pk1: e.tensor_tensor(tmp2[0:64, :], pk1[0:64, :], cosT[:], ALU.mult), [pk1B, cosB], [tmp2B])
                    V(lambda e, gs=gs: e.tensor_tensor(krT[:, gs], tmpf[0:64, :], tmp2[0:64, :], ALU.add), [tmpfB, tmp2B], [krTB[g]])
                    nkb = 4 * g + 4
                    ACC = (0, 1, 2, 3)
                    qst = {}

                    def qproj1(h):
                        hb = h % 2
                        pn, pnB = bank(ACC)
                        for qc in range(2):
                            P(lambda e, qc=qc: e.matmul(pn[:], wuq[:, qc, h, 0:128], cqT[:, qc, :], start=(qc == 0), stop=(qc == 1)), [wuqB, cqTB], [pnB])
                        A(lambda e: e.copy(qnT[:, hb, :], pn[:]), [pnB], [qnTB[hb]])
                        pr1, pr1B = bank(ACC)
                        for qc in range(2):
                            P(lambda e, qc=qc: e.matmul(pr1[0:64, :], wuq[:, qc, h, 128:192], cqT[:, qc, :], start=(qc == 0), stop=(qc == 1)), [wuqB, cqTB], [pr1B])
                        V(lambda e: e.scalar_tensor_tensor(tmp2[0:64, :], pr1[0:64, :], MSCALE, cosT[:], ALU.mult, ALU.mult), [pr1B, cosB], [tmp2B])
                        pr2, pr2B = bank(ACC)
                        for qc in range(2):
                            P(lambda e, qc=qc: e.matmul(pr2[0:64, :], wuq[:, qc, h, 192:256], cqT[:, qc, :], start=(qc == 0), stop=(qc == 1)), [wuqB, cqTB], [pr2B])
                        V(lambda e: e.scalar_tensor_tensor(tmpf[0:64, :], pr2[0:64, :], MSCALE, sinT[:], ALU.mult, ALU.mult), [pr2B, sinB], [tmpfB])
                        V(lambda e: e.tensor_tensor(qrT[:, hb, :], tmpf[0:64, :], tmp2[0:64, :], ALU.add), [tmpfB, tmp2B], [qrTB[hb]])

                    def qproj2(h):
                        hb = h % 2
                        pl_, plB_ = bank(ACC)
                        P(lambda e: e.matmul(pl_[:], wukT[:, h, :], qnT[:, hb, :], start=True, stop=True), [wukTB, qnTB[hb]], [plB_])
                        A(lambda e: e.activation(qlatT[:, hb, :], pl_[:], AF.Copy, scale=MSCALE), [plB_], [qlatTB[hb]])

                    st_psc = {}
                    st_pi = {}

                    def stA(h, kb):
                        hb = h % 2
                        kg = kb // 4
                        c0 = max(kb - 4 * g, 0) * 128
                        psc, pscB = bank(ACC)
                        st_psc[(h, kb)] = (psc, pscB)
                        ks = slice(kb * 128, (kb + 1) * 128)
                        P(lambda e: e.matmul(psc[:, c0:512], ckvT[:, ks], qlatT[:, hb, c0:512], start=True, stop=False),
                          [ckvTB[kg], qlatTB[hb]], [pscB])
                        P(lambda e: e.matmul(psc[:, c0:512], krT[:, ks], qrT[:, hb, c0:512], start=False, stop=True),
                          [krTB[kg], qrTB[hb]], [pscB])

                    def stB(h, kb):
                        jd = kb - 4 * g
                        c0 = max(jd, 0) * 128
                        psc, pscB = st_psc.pop((h, kb))
                        pi = pt_i[0] % len(PT)
                        pt_i[0] += 1
                        st_pi[(h, kb)] = pi
                        A(lambda e: e.activation(PT[pi][:, c0:512], psc[:, c0:512], AF.Exp), [pscB], [PTB[pi]])
                        if jd >= 0:
                            G(lambda e: e.tensor_tensor(PT[pi][:, c0:c0 + 128], PT[pi][:, c0:c0 + 128], CMb("TRI"), ALU.mult), [PTB[pi], cmbB], [PTB[pi]])

                    def stC(h, kb):
                        kg = kb // 4
                        jd = kb - 4 * g
                        pi = st_pi.pop((h, kb))
                        for i in range(max(jd, 0), 4):
                            last = (kb == 4 * g + i)
                            P(lambda e, i=i, last=last: e.matmul(ps[i][:, 0:129], PT[pi][:, i * 128:(i + 1) * 128], Vaug[:, kb, 0:129],
                                                                 start=(kb == 0), stop=last), [PTB[pi], VaugB[kg]], [psB[i]])
                        if kb == nkb - 1:
                            stF(h)

                    def stF(h):
                        for i in range(4):
                            V(lambda e, i=i: e.reciprocal(rec_[:, i:i + 1], ps[i][:, 128:129]), [psB[i]], [rec_B])
                            V(lambda e, i=i: e.tensor_scalar(olat[:, i, :], ps[i][:, 0:128], rec_[:, i:i + 1], None, ALU.mult), [psB[i], rec_B], [olatB])
                        pb, pbB = bank(ACC)
                        pbb = pb[:].bitcast(BF16)
                        for i in range(4):
                            P(lambda e, i=i: e.transpose(pbb[:, i * 128:(i + 1) * 128], olat[:, i, :], CMb("ident")), [olatB, cmbB], [pbB])
                        V(lambda e: e.tensor_copy(olT[:], pbb[:, 0:512]), [pbB], [olTB])
                        pv_, pvB_ = bank(ACC)
                        P(lambda e: e.matmul(pv_[:], wuv[:, h, :], olT[:], start=True, stop=True), [wuvB, olTB], [pvB_])
                        A(lambda e: e.copy(oT[:, h, :], pv_[:]), [pvB_], [oTB])

                    steps = [(h, kb) for h in range(8) for kb in range(nkb)]
                    qproj1(0)
                    qproj2(0)
                    LA_B, LA_C = 1, 3
                    for n in range(len(steps) + LA_C):
                        if 0 <= n - LA_B < len(steps):
                            stB(*steps[n - LA_B])
                        if n < len(steps):
                            h, kb = steps[n]
                            if kb == 0 and h + 1 < 8:
                                qproj1(h + 1)
                            if kb == 2 and h + 1 < 8:
                                qproj2(h + 1)
                            stA(h, kb)
                        if 0 <= n - LA_C < len(steps):
                            stC(*steps[n - LA_C])
                    for j in range(4):
                        t = g * 4 + j
                        js = slice(j * 128, (j + 1) * 128)
                        for half in range(2):
                            px, pxB = bank()
                            for h in range(8):
                                P(lambda e, h=h, px=px, half=half, js=js: e.matmul(px[:], oT[:, h, js], wo[:, h, half * 512:(half + 1) * 512], start=(h == 0), stop=(h == 7)),
                                  [oTB, woB], [pxB])
                            V(lambda e, px=px, half=half, j=j: e.tensor_tensor(x3t[:, half * 512:(half + 1) * 512], px[:], x2[j][:, half * 512:(half + 1) * 512], ALU.add),
                              [pxB, x2B[j]], [x3tB])
                        S_.dma("sp", X3_d[t * 128:(t + 1) * 128, :], x3t[:], reads=[x3tB], writes=[X3B[t]])
                        moe_pre(1, t, x3t, x3tB, W)
                S_.barrier()

        def phase_final():
            with contextlib.ExitStack() as st:
                xold = [sbt(st, "f_xold%d" % i, [128, D], F32) for i in range(2)]
                xoldB = [Buf(), Buf()]
                ya = [[sbt(st, "f_ya%d_%d" % (i, k), [128, D], F32) for k in range(2)] for i in range(2)]
                yaB = [[Buf(), Buf()] for i in range(2)]
                x4 = sbt(st, "f_x4", [128, D], F32); x4B = Buf()
                junk = sbt(st, "f_junk", [128, D], F32); junkB = Buf()
                ss = sbt(st, "f_ss", [128, 1], F32); ssB = Buf()
                rstd = sbt(st, "f_rstd", [128, 1], F32); rstdB = Buf()
                yo = [sbt(st, "f_yo%d" % i, [128, D], F32) for i in range(2)]
                yoB = [Buf(), Buf()]
                for t in range(NT):
                    i = t % 2
                    S_.dma("sp", xold[i][:], X3_d[t * 128:(t + 1) * 128, :], reads=[X3B[t]], writes=[xoldB[i]])
                    combine(1, t, xold[i], xoldB[i], ya[i], yaB[i], x4, x4B)
                    rms_stats((junk, junkB, ss, ssB, rstd, rstdB), x4[:], x4B, D, "f")
                    V(lambda e, i=i: e.scalar_tensor_tensor(yo[i][:], x4[:], rstd[:], gfin[:], ALU.mult, ALU.mult), [x4B, rstdB, gfinB], [yoB[i]])
                    S_.dma("act", out_d[t * 128:(t + 1) * 128, :], yo[i][:], reads=[yoB[i]], writes=[outB[t]])

        phase_gla()
        dispatch(0)
        experts(0)
        phase_mla()
        dispatch(1)
        experts(1)
        phase_final()
        S_.finish()
        print("bass program: instructions", S_.n_ins, "waits", S_.n_wait, flush=True)
    return nc, (cmat_np, cvec_np, tokidx_np)


_CACHE = {}


def kernel(**inputs):
    x = np.asarray(inputs["x"])
    B, S, _ = x.shape
    BLK = 512 if S >= 2048 else 128
    key = (S, BLK)
    if key not in _CACHE:
        _CACHE[key] = build_nc(S, BLK)
    nc, (cmat, cvec, tokidx) = _CACHE[key]
    names = ["attn_norm", "ffn_norm", "final_norm", "gla_w_in", "gla_w_gate", "gla_b_gate", "gla_out_norm", "gla_w_o",
             "mla_w_in", "mla_q_norm", "mla_w_uq", "mla_kv_norm", "mla_w_uk", "mla_w_uv", "mla_w_o",
             "moe_w_group", "moe_w_expert", "moe_w1", "moe_w3", "moe_w2"]
    shared = {n: np.ascontiguousarray(np.asarray(inputs[n], dtype=np.float32)) for n in names}
    shared["cmat"] = cmat
    shared["cvec"] = cvec
    shared["tokidx"] = tokidx
    pos = np.asarray(inputs["positions"]).astype(np.int32)
    in_maps = []
    for b in range(B):
        m = dict(shared)
        m["x"] = np.ascontiguousarray(x[b], dtype=np.float32)
        m["positions"] = np.ascontiguousarray(pos[b])
        in_maps.append(m)
    res = run_bass_kernel_spmd(nc, in_maps, core_ids=list(range(B)))
    out = np.stack([np.asarray(r["out"], dtype=np.float32) for r in res.results], axis=0)
    return out
```
